# Optimizing a Trainium2 kernel written in Bass

```python
import jax, jax.numpy as jnp
from jax import lax
import numpy as np

D_MODEL = 2048
BATCH = 16
SEQ = 2048
DEPTH = 4

MIX_HALF = D_MODEL // 2
RET_HEADS = 4
RET_HEAD_DIM = MIX_HALF // RET_HEADS
RET_CHUNK = 128
RET_THETA = 10000.0
SGU_GROUPS = 4
SGU_GROUP_DIM = MIX_HALF // SGU_GROUPS
SGU_CHUNK = 128
POOL_WINDOWS = (2, 4, 8, 16)
POOL_GROUP_DIM = MIX_HALF // len(POOL_WINDOWS)
MOBA_HEADS = 8
MOBA_HEAD_DIM = MIX_HALF // MOBA_HEADS
MOBA_BLOCK = 256
MOBA_TOPK = 3
MOBA_QUERY_CHUNK = 4
ROPE_THETA = 500000.0
ROT_DIM = MOBA_HEAD_DIM // 4
D_FF = 5632
PLE_DIM = 256
N_NORMS = 8
N_EVEN = (DEPTH + 1) // 2
N_ODD = DEPTH // 2
EPS = 1e-6
NEG = -1e30

kernel_name = "hybrid_retention_sgu_pool_moba_macaron"


def rms_norm(x, gain=None):
    xf = x.astype(jnp.float32)
    y = xf * lax.rsqrt(jnp.mean(xf * xf, axis=-1, keepdims=True) + EPS)
    if gain is not None:
        y = y * gain.astype(jnp.float32)
    return y.astype(x.dtype)


def rotary(x, rot_dim, theta):
    s = x.shape[1]
    half = rot_dim // 2
    inv = 1.0 / jnp.power(jnp.float32(theta), jnp.arange(half, dtype=jnp.float32) / half)
    ang = jnp.arange(s, dtype=jnp.float32)[:, None] * inv[None, :]
    cos = jnp.cos(ang)[None, :, None, :]
    sin = jnp.sin(ang)[None, :, None, :]
    xf = x.astype(jnp.float32)
    x1 = xf[..., :half]
    x2 = xf[..., half:rot_dim]
    out = jnp.concatenate([x1 * cos - x2 * sin, x2 * cos + x1 * sin, xf[..., rot_dim:]], axis=-1)
    return out.astype(x.dtype)


def swiglu(h, wg, wu, wd):
    return (jax.nn.silu(h @ wg) * (h @ wu)) @ wd


def retention(q, k, v, g):
    b, s, _ = q.shape
    h, d, c = RET_HEADS, RET_HEAD_DIM, RET_CHUNK
    nc = s // c
    q = rotary(q.reshape(b, s, h, d), d, RET_THETA)
    k = rotary(k.reshape(b, s, h, d), d, RET_THETA) * (d ** -0.5)
    v = v.reshape(b, s, h, d)
    to_chunks = lambda t: t.astype(jnp.float32).reshape(b, nc, c, h, d).transpose(1, 0, 3, 2, 4)
    log_gamma = jnp.log(1.0 - jnp.power(2.0, -5.0 - jnp.arange(h, dtype=jnp.float32)))
    pos = jnp.arange(c, dtype=jnp.float32)
    rel = pos[:, None] - pos[None, :]
    intra_decay = jnp.exp(jnp.maximum(rel, 0.0)[None] * log_gamma[:, None, None]) * (rel >= 0)[None]
    q_decay = jnp.exp((pos + 1.0)[None, :] * log_gamma[:, None])[..., None]
    k_decay = jnp.exp((c - 1.0 - pos)[None, :] * log_gamma[:, None])[..., None]
    chunk_decay = jnp.exp(c * log_gamma)[:, None, None]

    def step(state, qkv):
        qc, kc, vc = qkv
        scores = jnp.einsum('bhtd,bhsd->bhts', qc, kc) * intra_decay
        out = jnp.einsum('bhts,bhse->bhte', scores, vc) + jnp.einsum('bhtd,bhde->bhte', qc * q_decay, state)
        state = state * chunk_decay + jnp.einsum('bhsd,bhse->bhde', kc * k_decay, vc)
        return state, out

    state0 = jnp.zeros((b, h, d, d), jnp.float32)
    _, o = lax.scan(step, state0, (to_chunks(q), to_chunks(k), to_chunks(v)))
    o = rms_norm(o.transpose(1, 0, 3, 2, 4).reshape(b, s, h, d))
    return (o.reshape(b, s, h * d) * jax.nn.silu(g.astype(jnp.float32))).astype(g.dtype)


def spatial_gating(u, v, w_s, b_s, v_gain):
    b, s, _ = u.shape
    gr, dg, c = SGU_GROUPS, SGU_GROUP_DIM, SGU_CHUNK
    nc = s // c
    u = jax.nn.gelu(u)
    v = rms_norm(jax.nn.gelu(v).reshape(b, s, gr, dg), v_gain.reshape(gr, dg))
    w = w_s * jnp.tril(jnp.ones((c, c), w_s.dtype))[None]
    sv = jnp.einsum('gts,bcsgd->bctgd', w, v.reshape(b, nc, c, gr, dg)) + b_s.T[None, None, :, :, None]
    return (u.reshape(b, nc, c, gr, dg) * sv).reshape(b, s, gr * dg)


def pool_mixer(z, w_pool, scale):
    b, s, _ = z.shape
    zf = z.astype(jnp.float32).reshape(b, s, len(POOL_WINDOWS), POOL_GROUP_DIM)
    cs = jnp.cumsum(zf, axis=1)
    t = jnp.arange(s)
    outs = []
    for gi, w in enumerate(POOL_WINDOWS):
        c = cs[:, :, gi]
        prev = jnp.pad(c, ((0, 0), (w, 0), (0, 0)))[:, :s]
        cnt = jnp.minimum(t + 1, w).astype(jnp.float32)
        outs.append((c - prev) / cnt[None, :, None] - zf[:, :, gi])
    y = jnp.stack(outs, axis=2).astype(z.dtype)
    y = jnp.einsum('bsgc,gcd->bsgd', y, w_pool).reshape(b, s, -1)
    return y * scale


def moba(q, k, v):
    b, s, _ = q.shape
    h, hd, bs, qc_len = MOBA_HEADS, MOBA_HEAD_DIM, MOBA_BLOCK, MOBA_QUERY_CHUNK
    nb = -(-s // bs)
    s_pad = nb * bs
    nq = s // qc_len
    n_sel = min(MOBA_TOPK, nb)
    q = rotary(q.reshape(b, s, h, hd), ROT_DIM, ROPE_THETA).transpose(0, 2, 1, 3)
    k = rotary(k.reshape(b, s, h, hd), ROT_DIM, ROPE_THETA).transpose(0, 2, 1, 3)
    v = v.reshape(b, s, h, hd).transpose(0, 2, 1, 3)
    pad = ((0, 0), (0, 0), (0, s_pad - s), (0, 0))
    kb = jnp.pad(k, pad).reshape(b, h, nb, bs, hd)
    vb = jnp.pad(v, pad).reshape(b, h, nb, bs, hd)
    k_mean = jnp.mean(kb.astype(jnp.float32), axis=3)
    t = jnp.arange(s)
    q_block = t // bs
    past = jnp.arange(nb)[None, :] < q_block[:, None]
    gate = jnp.einsum('bhsd,bhnd->bhsn', q.astype(jnp.float32), k_mean)
    gate = jnp.where(past, gate, NEG)
    _, top = lax.top_k(gate, n_sel)
    own = jnp.broadcast_to(q_block.astype(top.dtype)[None, None, :, None], (b, h, s, 1))
    idx = jnp.concatenate([top, own], axis=-1)
    slot_ok = jnp.arange(n_sel)[None, :] < q_block[:, None]
    causal = jnp.arange(bs)[None, :] <= (t % bs)[:, None]
    mask = jnp.concatenate([jnp.broadcast_to(slot_ok[:, :, None], (s, n_sel, bs)), causal[:, None, :]], axis=1)
    qcs = q.reshape(b, h, nq, qc_len, hd).transpose(2, 0, 1, 3, 4)
    ics = idx.reshape(b, h, nq, qc_len, n_sel + 1).transpose(2, 0, 1, 3, 4)
    mcs = mask.reshape(nq, qc_len, n_sel + 1, bs)
    gather_blocks = jax.vmap(jax.vmap(lambda blocks, ids: blocks[ids]))
    scale = hd ** -0.5

    def attend(args):
        qc, ic, mc = args
        kg = gather_blocks(kb, ic)
        vg = gather_blocks(vb, ic)
        logits = jnp.einsum('bhqd,bhqnkd->bhqnk', qc, kg).astype(jnp.float32) * scale
        logits = jnp.where(mc, logits, NEG)
        pr = jax.nn.softmax(logits.reshape(b, h, qc_len, -1), axis=-1).astype(vg.dtype)
        return jnp.einsum('bhqk,bhqkd->bhqd', pr, vg.reshape(b, h, qc_len, -1, hd))

    o = lax.map(attend, (qcs, ics, mcs))
    return o.transpose(1, 0, 3, 2, 4).reshape(b, s, h * hd)


def even_mixer(h, w_in, w_out, sgu_w, sgu_b, sgu_gain):
    z = h @ w_in
    q, k, v, g, u, vs = jnp.split(z, 6, axis=-1)
    y = jnp.concatenate([retention(q, k, v, g), spatial_gating(u, vs, sgu_w, sgu_b, sgu_gain)], axis=-1)
    return y @ w_out


def odd_mixer(h, w_in, w_out, pool_w, pool_scale):
    z = h @ w_in
    pz, q, k, v = jnp.split(z, 4, axis=-1)
    y = jnp.concatenate([pool_mixer(pz, pool_w, pool_scale), moba(q, k, v)], axis=-1)
    return y @ w_out


def setup_inputs(seed: int = 0) -> dict:
    key = jax.random.key(seed)
    ks = jax.random.split(key, 17)
    nrm = jax.random.normal
    f32 = jnp.float32
    return {
        "x": nrm(ks[0], (BATCH, SEQ, D_MODEL), f32),
        "p": nrm(ks[1], (DEPTH, BATCH, SEQ, PLE_DIM), f32),
        "norm_gains": 1.0 + 0.1 * nrm(ks[2], (DEPTH, N_NORMS, D_MODEL), f32),
        "w_ffn_gate": nrm(ks[3], (DEPTH, 2, D_MODEL, D_FF), f32) * D_MODEL ** -0.5,
        "w_ffn_up": nrm(ks[4], (DEPTH, 2, D_MODEL, D_FF), f32) * D_MODEL ** -0.5,
        "w_ffn_down": nrm(ks[5], (DEPTH, 2, D_FF, D_MODEL), f32) * D_FF ** -0.5,
        "w_in_even": nrm(ks[6], (N_EVEN, D_MODEL, 6 * MIX_HALF), f32) * D_MODEL ** -0.5,
        "w_out_even": nrm(ks[7], (N_EVEN, 2 * MIX_HALF, D_MODEL), f32) * (2 * MIX_HALF) ** -0.5,
        "sgu_w": nrm(ks[8], (N_EVEN, SGU_GROUPS, SGU_CHUNK, SGU_CHUNK), f32) * SGU_CHUNK ** -0.5,
        "sgu_b": 1.0 + 0.1 * nrm(ks[9], (N_EVEN, SGU_GROUPS, SGU_CHUNK), f32),
        "sgu_gain": 1.0 + 0.1 * nrm(ks[10], (N_EVEN, MIX_HALF), f32),
        "w_in_odd": nrm(ks[11], (N_ODD, D_MODEL, 4 * MIX_HALF), f32) * D_MODEL ** -0.5,
        "w_out_odd": nrm(ks[12], (N_ODD, 2 * MIX_HALF, D_MODEL), f32) * (2 * MIX_HALF) ** -0.5,
        "pool_w": nrm(ks[13], (N_ODD, len(POOL_WINDOWS), POOL_GROUP_DIM, POOL_GROUP_DIM), f32) * POOL_GROUP_DIM ** -0.5,
        "pool_scale": 1.0 + 0.1 * nrm(ks[14], (N_ODD, MIX_HALF), f32),
        "w_ple_gate": nrm(ks[15], (DEPTH, D_MODEL, D_MODEL), f32) * D_MODEL ** -0.5,
        "w_ple_proj": nrm(ks[16], (DEPTH, PLE_DIM, D_MODEL), f32) * PLE_DIM ** -0.5,
    }


def reference(x, p, norm_gains, w_ffn_gate, w_ffn_up, w_ffn_down, w_in_even, w_out_even, sgu_w, sgu_b,
              sgu_gain, w_in_odd, w_out_odd, pool_w, pool_scale, w_ple_gate, w_ple_proj):
    for i in range(DEPTH):
        ng = norm_gains[i]
        h = rms_norm(x, ng[0])
        x = x + 0.5 * rms_norm(swiglu(h, w_ffn_gate[i, 0], w_ffn_up[i, 0], w_ffn_down[i, 0]), ng[1])
        h = rms_norm(x, ng[2])
        if i % 2 == 0:
            j = i // 2
            m = even_mixer(h, w_in_even[j], w_out_even[j], sgu_w[j], sgu_b[j], sgu_gain[j])
        else:
            j = i // 2
            m = odd_mixer(h, w_in_odd[j], w_out_odd[j], pool_w[j], pool_scale[j])
        x = x + rms_norm(m, ng[3])
        h = rms_norm(x, ng[4])
        x = x + 0.5 * rms_norm(swiglu(h, w_ffn_gate[i, 1], w_ffn_up[i, 1], w_ffn_down[i, 1]), ng[5])
        gate = jax.nn.sigmoid(rms_norm(x, ng[6]) @ w_ple_gate[i])
        x = x + rms_norm(gate * (p[i] @ w_ple_proj[i]), ng[7])
    return x
```

```python
import math
from contextlib import ExitStack
import numpy as np
import concourse.bass as bass
import concourse.mybir as mybir
from concourse.bass_utils import run_bass_kernel_spmd

F32 = mybir.dt.float32
BF16 = mybir.dt.bfloat16
AF = mybir.ActivationFunctionType
ALU = mybir.AluOpType
AX = mybir.AxisListType

D = 2048
DFF = 5632
S = 2048
TT = 512
KC = 16
FC = 44
EPS = 1e-6
NEGB = -30000.0
RING = 4
RSZ = 5632
import os
DBG_STOP = int(os.environ.get("KDBG_STOP", "99"))


class Sched:
    def __init__(self, nc, es):
        self.nc = nc
        self.es = es
        self.E = {"pe": nc.tensor, "act": nc.scalar, "dve": nc.vector, "pool": nc.gpsimd, "sp": nc.sync}
        self.sem = {}
        self.cnt = {}
        self.last_w = {}
        self.rd = {}
        self.seen = {k: {} for k in self.E}
        self.last_sig = {}
        self.nops = 0

    def _sem(self, key):
        if key not in self.sem:
            self.sem[key] = self.es.enter_context(self.nc.semaphore("s_" + "_".join(str(k) for k in key)))
            self.cnt[key] = 0
        return self.sem[key]

    def _wait(self, eng, key, val):
        if self.seen[eng].get(key, 0) >= val:
            return
        self.E[eng].wait_ge(self._sem(key), val)
        self.seen[eng][key] = val

    def op(self, eng, fn, r=(), w=(), dma=None, ndma=1):
        if dma is not None and dma[0] in ("misc", "cast") and ("dma", dma) in self.cnt:
            self._wait(eng, ("dma", dma), self.cnt[("dma", dma)])
        deps = {}
        for k in r:
            if k in self.last_w:
                sk, v = self.last_w[k]
                deps[sk] = max(deps.get(sk, 0), v)
        for k in w:
            if k in self.last_w:
                sk, v = self.last_w[k]
                deps[sk] = max(deps.get(sk, 0), v)
            for sk, v in self.rd.get(k, {}).items():
                deps[sk] = max(deps.get(sk, 0), v)
        mykey = ("dma", dma) if dma is not None else ("eng", eng)
        for sk, v in deps.items():
            if sk == ("eng", "pe") and eng == "pe" and dma is None:
                continue
            self._wait(eng, sk, v)
        res = fn(self.E[eng])
        sem = self._sem(mykey)
        if dma is not None:
            assert len(res) == ndma
            for ins in res:
                ins.then_inc(sem, 16)
            self.cnt[mykey] += 16 * ndma
        else:
            last = res[-1] if isinstance(res, (list, tuple)) else res
            last.then_inc(sem, 1)
            self.cnt[mykey] += 1
        val = self.cnt[mykey]
        self.last_sig[mykey] = val
        for k in r:
            d = self.rd.setdefault(k, {})
            d[mykey] = val
        for k in w:
            self.last_w[k] = (mykey, val)
            self.rd[k] = {}
        self.nops += 1

    def barrier(self, engines=("pe", "act", "dve", "pool", "sp")):
        for eng in engines:
            for sk, v in self.last_sig.items():
                self._wait(eng, sk, v)


def build_program(NB, layers, n_stage=4, debug_out=False):
    NT = NB * S
    NTT = NT // TT
    import os
    if os.environ.get("KDBG_NTT"):
        NTT = int(os.environ["KDBG_NTT"])
    nc = bass.Bass("TRN2", target_bir_lowering=False)
    es = ExitStack()
    with es:
        def din(name, shape, dt=F32):
            return nc.dram_tensor(name, list(shape), dt, kind="ExternalInput").ap()
        x_in = din("x", [NT, D])
        p_in = din("p", [4, NT, 256])
        ng_in = din("norm_gains", [4, 8, D])
        y_out = nc.dram_tensor("out", [NT, D], F32, kind="ExternalOutput").ap()
        def dbf(name, shape):
            return nc.dram_tensor(name, list(shape), BF16).ap()
        gu_in, wd_in, gu_b, wd_b, pg_in, pp_in, pg_b, pp_b = {}, {}, {}, {}, {}, {}, {}, {}
        wi1_in, wi2_in, wo_in, wi1_b, wi2_b, wo_b = {}, {}, {}, {}, {}, {}
        for li in layers:
            for j in range(2):
                f = li * 2 + j
                gu_in[f] = din(f"w_gu{f}", [FC, 128, 4096])
                wd_in[f] = din(f"w_dn{f}", [16, 128, RSZ])
                gu_b[f] = dbf(f"b_gu{f}", [FC, 128, 4096])
                wd_b[f] = dbf(f"b_dn{f}", [16, 128, RSZ])
            pg_in[li] = din(f"w_pg{li}", [8, 128, 4096])
            pp_in[li] = din(f"w_pp{li}", [128, 4096])
            pg_b[li] = dbf(f"b_pg{li}", [8, 128, 4096])
            pp_b[li] = dbf(f"b_pp{li}", [128, 4096])

        ev_layers = [li for li in layers if li % 2 == 0]
        wi1_in, wi2_in, wo_in, wi1_b, wi2_b, wo_b = {}, {}, {}, {}, {}, {}
        sguw_in, sgub_in, sgug_in = {}, {}, {}
        for li in layers:
            n1, n2 = (8, 16) if li % 2 == 0 else (8, 8)
            wi1_in[li] = din(f"w_i1_{li}", [n1, 128, 4096])
            wi2_in[li] = din(f"w_i2_{li}", [n2, 128, 4096])
            wo_in[li] = din(f"w_o_{li}", [8, 128, 4096])
            wi1_b[li] = dbf(f"b_i1_{li}", [n1, 128, 4096])
            wi2_b[li] = dbf(f"b_i2_{li}", [n2, 128, 4096])
            wo_b[li] = dbf(f"b_o_{li}", [8, 128, 4096])
            if li % 2 == 0:
                sguw_in[li] = din(f"sgu_wT_{li}", [128, 4, 128])
                sgub_in[li] = din(f"sgu_bT_{li}", [128, 4])
                sgug_in[li] = din(f"sgu_g_{li}", [1, 1024])
        poolw_in, pools_in = {}, {}
        for li in layers:
            if li % 2 == 1:
                poolw_in[li] = din(f"pool_w_{li}", [128, 8, 256])
                pools_in[li] = din(f"pool_s_{li}", [1, 1024])
        c_rot2 = din("c_rot2", [2, 128, S])
        c_perm = din("c_perm", [128, 128])
        c_ct = din("c_ct", [4, 128, TT])
        c_ind = din("c_ind", [8, 8 * 128])
        c_negm = din("c_negm", [1, 64])
        c_bcon = din("c_bcon", [1, 32])
        c_pool = din("c_pool", [12, 128, 128])
        pv_d = dbf("pv_d", [NT, D])
        km_d = dbf("km_d", [128, 64])
        c_rot = din("c_rot", [2, 128, S])
        c_g2 = din("c_g2", [4, 128, 1024])
        c_tri = din("c_tri", [128, 128])
        qk_d = dbf("qk_d", [16, 128, NT])
        vg_d = dbf("vg_d", [NT, 4096])
        y_d = dbf("y_d", [NT, D])
        sc = Sched(nc, es)
        ring = es.enter_context(nc.sbuf_tensor("ring", [128, RING, RSZ], BF16))
        ident = es.enter_context(nc.sbuf_tensor("ident", [128, 128], BF16))
        identf = es.enter_context(nc.sbuf_tensor("identf", [128, 128], F32))
        psum = es.enter_context(nc.psum_tensor("psum", [128, 8, 512], F32))
        psb = psum[:].bitcast(BF16)

        epsc = es.enter_context(nc.sbuf_tensor("epsc", [128, 4], F32))
        sc.op("dve", lambda E: E.memset(epsc[:], EPS), w=["epsc"])
        sc.op("pool", lambda E: E.memset(identf[:], 0.0), w=["identf"])
        sc.op("pool", lambda E: E.affine_select(out=identf[:], in_=identf[:], pattern=[[-1, 128]],
                                                compare_op=ALU.not_equal, fill=1.0, base=0, channel_multiplier=1),
              w=["identf"])
        sc.op("dve", lambda E: E.tensor_copy(out=ident[:], in_=identf[:]), r=["identf"], w=["ident"])

        cast_done = set()
        cast_n = [0]
        def cast(key, dst, src):
            if key in cast_done:
                return
            cast_done.add(key)
            n = dst.shape[0] if len(dst.shape) == 3 else 1
            def f(E):
                if n == 1:
                    return [E.dma_start(out=dst, in_=src)]
                return [E.dma_start(out=dst[c], in_=src[c]) for c in range(n)]
            cast_n[0] += 1
            sc.op("pool", f, w=[("wb", key)], dma=("cast", cast_n[0] % 2), ndma=n)

        ring_state = {"n": 0}
        def ring_load(src_ap, nel, wkey):
            i = ring_state["n"] % RING
            ring_state["n"] += 1
            sc.op("sp", lambda E: [E.dma_start(out=ring[:, i, 0:nel], in_=src_ap)],
                  r=[("wb", wkey)], w=[("ring", i)], dma=("ring", i))
            return i

        uid = [0]
        bank_state = {"n": 0}
        def next_bank(lo=0, hi=8):
            b = lo + bank_state["n"] % (hi - lo)
            bank_state["n"] += 1
            return b

        def transpose_T(src_fn, src_keys, hT, s, hp=0):
            for half in range(2):
                b = next_bank(0, 4)
                def tr(E, half=half, b=b):
                    out = []
                    for q in range(8):
                        kc = half * 8 + q
                        out.append(E.transpose(out=psb[:, b, q * 128:(q + 1) * 128], in_=src_fn(kc), identity=ident[:]))
                    return out
                sc.op("pe", tr, r=list(src_keys) + ["ident"], w=[("ps", b)])
                eng = "act" if half == 0 else "dve"
                def ev(E, half=half, b=b, eng=eng):
                    src = psb[:, b, :].rearrange("p (q t) -> p q t", q=8)
                    dst = hT[:, half * 8:(half + 1) * 8, s * 128:(s + 1) * 128]
                    if eng == "act":
                        return E.copy(out=dst, in_=src)
                    return E.tensor_copy(out=dst, in_=src)
                sc.op(eng, ev, r=[("ps", b)], w=[("hT", hp, s, half)])

        def norm_parts(src_dram, t0, XB, xn, hT, gin, stats, junk, nxb=4, hp=0, ioq="sp"):
            def zero():
                sc.op("dve", lambda E: E.memset(stats[:, 0:4], 0.0), w=[("ss", s) for s in range(4)])
            parts = []
            for s in range(4):
                def norm_s(s=s):
                    r0 = t0 + s * 128
                    xs = s % nxb
                    j = s % 2
                    sc.op(ioq, lambda E: [E.dma_start(out=XB[:, xs, :], in_=src_dram[r0:r0 + 128, :])],
                          r=[("xd", r0 // 128)], w=[("XB", xs)], dma=("XB", xs))
                    sc.op("act", lambda E: E.activation(out=junk[:], in_=XB[:, xs, :], func=AF.Square,
                                                        accum_out=stats[:, s:s + 1]),
                          r=[("XB", xs)], w=["junk", ("ss", s)])
                    sc.op("act", lambda E: E.activation(out=stats[:, 8 + s:9 + s], in_=stats[:, s:s + 1], func=AF.Sqrt,
                                                        bias=epsc[:, 0:1], scale=1.0 / D),
                          r=[("ss", s), "epsc"], w=[("rs_a", s)])
                    sc.op("dve", lambda E: E.reciprocal(out=stats[:, 16 + s:17 + s], in_=stats[:, 8 + s:9 + s]),
                          r=[("rs_a", s)], w=[("rs", s)])
                    sc.op("dve", lambda E: E.scalar_tensor_tensor(
                        out=xn[:, j, :], in0=XB[:, xs, :], scalar=stats[:, 16 + s:17 + s], in1=gin[:],
                        op0=ALU.mult, op1=ALU.mult),
                        r=[("XB", xs), ("rs", s), "gin"], w=[("xn", j)])
                def tr_s(s=s):
                    j = s % 2
                    transpose_T(lambda kc: xn[:, j, kc * 128:(kc + 1) * 128], [("xn", j)], hT, s, hp)
                parts.append((norm_s, tr_s))
            return zero, parts

        def load_norm_T(st, src_dram, t0, gain_row, XB, xn, hT, gin, stats, junk, nxb=4, hp=0, ioq="sp"):
            zero, parts = norm_parts(src_dram, t0, XB, xn, hT, gin, stats, junk, nxb, hp, ioq)
            zero()
            for nfn, tfn in parts:
                nfn()
                tfn()

        def hT_keys(hp=0):
            return [("hT", hp, s, h) for s in range(4) for h in range(2)]

        def epilogue_parts(t0, XB, XS, gout, stats, junk, src_dram, coef, ioq="sp"):
            parts = []
            for s in range(4):
                def part(s=s):
                    r0 = t0 + s * 128
                    j = s % 2
                    sc.op(ioq, lambda E: [E.dma_start(out=XS[:, j, :], in_=src_dram[r0:r0 + 128, :])],
                          r=[("xd", r0 // 128)], w=[("XS", j)], dma=("XS", j))
                    sc.op("dve", lambda E: E.tensor_reduce(out=stats[:, 40 + s:41 + s],
                                                           in_=stats[:, 24 + 4 * s:28 + 4 * s],
                                                           axis=AX.X, op=ALU.add),
                          r=[("ssp", s, n) for n in range(4)], w=[("sso", s)])
                    sc.op("act", lambda E: E.activation(out=stats[:, 44 + s:45 + s], in_=stats[:, 40 + s:41 + s],
                                                        func=AF.Sqrt, bias=epsc[:, 0:1], scale=1.0 / D),
                          r=[("sso", s), "epsc"], w=[("rso_a", s)])
                    sc.op("dve", lambda E: E.reciprocal(out=stats[:, 48 + s:49 + s], in_=stats[:, 44 + s:45 + s]),
                          r=[("rso_a", s)], w=[("rso", s)])
                    sc.op("dve", lambda E: E.scalar_tensor_tensor(
                        out=XB[:, s, :], in0=XB[:, s, :], scalar=stats[:, 48 + s:49 + s], in1=gout[:],
                        op0=ALU.mult, op1=ALU.mult),
                        r=[("rso", s), "gout"], w=[("XB", s)])
                    sc.op("dve", lambda E: E.scalar_tensor_tensor(
                        out=XS[:, j, :], in0=XB[:, s, :], scalar=float(coef), in1=XS[:, j, :],
                        op0=ALU.mult, op1=ALU.add),
                        r=[("XB", s)], w=[("XS", j)])
                    sc.op(ioq, lambda E: [E.dma_start(out=y_out[r0:r0 + 128, :], in_=XS[:, j, :])],
                          r=[("XS", j)], w=[("xd", r0 // 128)], dma=("XSst", j))
                parts.append(part)
            return parts

        def epilogue(t0, XB, XS, gout, stats, junk, src_dram, coef, ioq="sp"):
            for p_ in epilogue_parts(t0, XB, XS, gout, stats, junk, src_dram, coef, ioq):
                p_()

        def linear_N(hT_ap_fn, nk, src_chunks, wkey, n_cols_chunks, kper, consume, in_keys):
            ng = nk // kper
            for n in range(n_cols_chunks):
                banks = [next_bank() for _ in range(4)]
                for g in range(ng):
                    slot = ring_load(src_chunks[n * ng + g], kper * 512, wkey)
                    for s in range(4):
                        def mm(E, s=s, g=g, slot=slot, b=banks[s]):
                            out = []
                            for q in range(kper):
                                kc = g * kper + q
                                out.append(E.matmul(psum[:, b, :], lhsT=hT_ap_fn(kc, s),
                                                    rhs=ring[:, slot, q * 512:(q + 1) * 512],
                                                    start=(kc == 0), stop=(kc == nk - 1)))
                            return out
                        sc.op("pe", mm, r=[("ring", slot)] + in_keys(s), w=[("ps", banks[s])])
                for s in range(4):
                    consume(n, s, banks[s])

        def ffn_stage(li, j, src_dram):
            f = li * 2 + j
            cast(("gu", f), gu_b[f], gu_in[f])
            cast(("dn", f), wd_b[f], wd_in[f])
            with ExitStack() as st:
                def sb(name, shape, dt):
                    uid[0] += 1
                    return st.enter_context(nc.sbuf_tensor(f"{name}_{uid[0]}", shape, dt))
                XB = sb("XB", [128, 4, D], F32)
                XS = sb("XS", [128, 2, D], F32)
                xn = sb("xn", [128, 2, D], BF16)
                hT2 = sb("hT", [128, 2, KC, TT], BF16)
                act = sb("act", [128, FC, TT], BF16)
                gin = sb("gin", [128, D], F32)
                gout = sb("gout", [128, D], F32)
                sg = sb("sg", [128, 2, TT], F32)
                junk = sb("junk", [128, D], BF16)
                stats = sb("stats", [128, 64], F32)
                gi = 0 if j == 0 else 4
                sc.op("sp", lambda E: [E.dma_start(out=gin[:], in_=ng_in[li, gi:gi + 1, :].partition_broadcast(128))],
                      w=["gin"], dma=("misc",))
                sc.op("sp", lambda E: [E.dma_start(out=gout[:], in_=ng_in[li, gi + 1:gi + 2, :].partition_broadcast(128))],
                      w=["gout"], dma=("misc",))
                load_norm_T(st, src_dram, 0, None, XB, xn, hT2[:, 0], gin, stats, junk, hp=0, ioq="pool")
                pend = None
                for tt in range(NTT):
                    t0 = tt * TT
                    hp = tt % 2
                    hT = hT2[:, hp]
                    pre = None
                    if tt + 1 < NTT:
                        pre = norm_parts(src_dram, t0 + TT, XB, xn, hT2[:, 1 - hp], gin, stats, junk, 4, 1 - hp, "pool")
                    sched = {}
                    if pend is not None:
                        for s_ in range(4):
                            sched.setdefault(1 + 2 * s_, []).append(pend[s_])
                    if pre is not None:
                        sched.setdefault(12, []).extend([pre[0], pre[1][0][0], pre[1][1][0]])
                        sched.setdefault(24, []).extend([pre[1][0][1], pre[1][1][1]])
                        sched.setdefault(26, []).extend([pre[1][2][0], pre[1][3][0]])
                    for fc in range(FC):
                        for fn_ in sched.get(fc, []):
                            fn_()
                        slot = ring_load(gu_b[f][fc], 4096, ("gu", f))
                        bg = next_bank()
                        bu = next_bank()
                        def mmgu(E, slot=slot, bg=bg, bu=bu):
                            out = []
                            for which, b in ((0, bg), (1, bu)):
                                for kc in range(KC):
                                    off = (which * KC + kc) * 128
                                    out.append(E.matmul(psum[:, b, :], lhsT=ring[:, slot, off:off + 128],
                                                        rhs=hT[:, kc, :], start=(kc == 0), stop=(kc == KC - 1)))
                            return out
                        sc.op("pe", mmgu, r=[("ring", slot)] + hT_keys(hp), w=[("ps", bg), ("ps", bu)])
                        jj = fc % 2
                        sc.op("act", lambda E, bg=bg, jj=jj: E.activation(out=sg[:, jj, :], in_=psum[:, bg, :], func=AF.Silu),
                              r=[("ps", bg)], w=[("sg", jj)])
                        sc.op("dve", lambda E, bu=bu, jj=jj, fc=fc: E.tensor_tensor(out=act[:, fc, :], in0=sg[:, jj, :],
                                                                                 in1=psum[:, bu, :], op=ALU.mult),
                              r=[("sg", jj), ("ps", bu)], w=[("act", fc)])
                    if pre is not None:
                        pre[1][2][1]()
                        pre[1][3][1]()
                    sc.op("dve", lambda E: E.memset(stats[:, 24:44], 0.0),
                          w=[("ssp", s, n) for s in range(4) for n in range(4)])
                    def consume(n, s, b):
                        sc.op("dve", lambda E: E.tensor_copy(out=XB[:, s, n * 512:(n + 1) * 512], in_=psum[:, b, :]),
                              r=[("ps", b)], w=[("XB", s)])
                        sc.op("act", lambda E: E.activation(out=junk[:, 0:512], in_=XB[:, s, n * 512:(n + 1) * 512],
                                                            func=AF.Square,
                                                            accum_out=stats[:, 24 + 4 * s + n:25 + 4 * s + n]),
                              r=[("XB", s)], w=["junk", ("ssp", s, n)])
                    chunks = [wd_b[f][c] for c in range(16)]
                    linear_N(lambda kc, s: act[:, kc, s * 128:(s + 1) * 128], FC, chunks, ("dn", f), 4, 11, consume,
                             lambda s: [("act", fc) for fc in range(FC)])
                    pend = epilogue_parts(t0, XB, XS, gout, stats, junk, src_dram, 0.5, "pool")
                for p_ in pend:
                    p_()
                sc.barrier()

        def ple_stage(li, src_dram):
            cast(("pg", li), pg_b[li], pg_in[li])
            cast(("pp", li), pp_b[li], pp_in[li])
            with ExitStack() as st:
                def sb(name, shape, dt):
                    uid[0] += 1
                    return st.enter_context(nc.sbuf_tensor(f"{name}_{uid[0]}", shape, dt))
                XB = sb("XB", [128, 4, D], F32)
                XS = sb("XS", [128, 2, D], F32)
                xn = sb("xn", [128, 2, D], BF16)
                hT2 = sb("hT", [128, 2, KC, TT], BF16)
                XL = sb("XL", [128, 2, D], F32)
                gin = sb("gin", [128, D], F32)
                gout = sb("gout", [128, D], F32)
                sgm = sb("sgm", [128, 2, TT], F32)
                junk = sb("junk", [128, D], BF16)
                stats = sb("stats", [128, 64], F32)
                wpp = sb("wpp", [128, 2, D], BF16)
                pt = sb("pt", [128, 2, 256], F32)
                ptb = sb("ptb", [128, 2, 256], BF16)
                pT = sb("pT", [128, 2, TT], BF16)
                sc.op("sp", lambda E: [E.dma_start(out=wpp[:], in_=pp_b[li].rearrange("p (k c) -> p k c", k=2))],
                      r=[("wb", ("pp", li))], w=["wpp"], dma=("misc",))
                sc.op("sp", lambda E: [E.dma_start(out=gin[:], in_=ng_in[li, 6:7, :].partition_broadcast(128))],
                      w=["gin"], dma=("misc",))
                sc.op("sp", lambda E: [E.dma_start(out=gout[:], in_=ng_in[li, 7:8, :].partition_broadcast(128))],
                      w=["gout"], dma=("misc",))
                load_norm_T(st, src_dram, 0, None, XL, xn, hT2[:, 0], gin, stats, junk, nxb=2, hp=0, ioq="pool")
                for tt in range(NTT):
                    t0 = tt * TT
                    hp = tt % 2
                    hT = hT2[:, hp]
                    sc.op("dve", lambda E: E.memset(stats[:, 24:44], 0.0),
                          w=[("ssp", s, n) for s in range(4) for n in range(4)])
                    for s in range(4):
                        r0 = t0 + s * 128
                        j = s % 2
                        sc.op("sp", lambda E, j=j, r0=r0: [E.dma_start(out=pt[:, j, :], in_=p_in[li, r0:r0 + 128, :])],
                              w=[("pt", j)], dma=("pt", j))
                        sc.op("pool", lambda E, j=j: E.tensor_copy(out=ptb[:, j, :], in_=pt[:, j, :]),
                              r=[("pt", j)], w=[("ptb", j)])
                        b = next_bank()
                        def tr(E, b=b, j=j):
                            return [E.transpose(out=psb[:, b, q * 128:(q + 1) * 128], in_=ptb[:, j, q * 128:(q + 1) * 128],
                                                identity=ident[:]) for q in range(2)]
                        sc.op("pe", tr, r=[("ptb", j), "ident"], w=[("ps", b)])
                        sc.op("act", lambda E, b=b, s=s: E.copy(out=pT[:, :, s * 128:(s + 1) * 128],
                                                                in_=psb[:, b, 0:256].rearrange("p (q t) -> p q t", q=2)),
                              r=[("ps", b)], w=[("pT", s)])
                    def consume(n, s, b):
                        jj = (n * 4 + s) % 2
                        sc.op("act", lambda E: E.activation(out=sgm[:, jj, :], in_=psum[:, b, :], func=AF.Sigmoid),
                              r=[("ps", b)], w=[("sgm", jj)])
                        b2 = next_bank()
                        def mm2(E):
                            return [E.matmul(psum[:, b2, :], lhsT=pT[:, q, s * 128:(s + 1) * 128],
                                             rhs=wpp[:, q, n * 512:(n + 1) * 512], start=(q == 0), stop=(q == 1))
                                    for q in range(2)]
                        sc.op("pe", mm2, r=[("pT", s), "wpp"], w=[("ps", b2)])
                        sc.op("dve", lambda E: E.tensor_tensor(out=XB[:, s, n * 512:(n + 1) * 512], in0=sgm[:, jj, :],
                                                               in1=psum[:, b2, :], op=ALU.mult),
                              r=[("sgm", jj), ("ps", b2)], w=[("XB", s)])
                        sc.op("act", lambda E: E.activation(out=junk[:, 0:512], in_=XB[:, s, n * 512:(n + 1) * 512],
                                                            func=AF.Square,
                                                            accum_out=stats[:, 24 + 4 * s + n:25 + 4 * s + n]),
                              r=[("XB", s)], w=["junk", ("ssp", s, n)])
                    chunks = [pg_b[li][c] for c in range(8)]
                    linear_N(lambda kc, s: hT[:, kc, s * 128:(s + 1) * 128], KC, chunks, ("pg", li), 4, 8, consume,
                             lambda s: hT_keys(hp))
                    if tt + 1 < NTT:
                        load_norm_T(st, src_dram, t0 + TT, None, XL, xn, hT2[:, 1 - hp], gin, stats, junk, nxb=2, hp=1 - hp, ioq="pool")
                    epilogue(t0, XB, XS, gout, stats, junk, src_dram, 1.0, "pool")
                sc.barrier()

        GAM = [1.0 - 2.0 ** (-5.0 - h) for h in range(4)]

        def gelu_ops(b, dst, dst_keys, gx, gt, gs, kk):
            sc.op("act", lambda E: E.copy(out=gx[:, kk, :], in_=psum[:, b, :]), r=[("ps", b)], w=[("gx", kk)])
            sc.op("pool", lambda E: E.tensor_tensor(out=gt[:, kk, :], in0=gx[:, kk, :], in1=gx[:, kk, :], op=ALU.mult),
                  r=[("gx", kk)], w=[("gt", kk)])
            sc.op("pool", lambda E: E.tensor_scalar(out=gt[:, kk, :], in0=gt[:, kk, :], scalar1=0.044715, scalar2=1.0,
                                                    op0=ALU.mult, op1=ALU.add), w=[("gt", kk)])
            sc.op("pool", lambda E: E.tensor_tensor(out=gt[:, kk, :], in0=gt[:, kk, :], in1=gx[:, kk, :], op=ALU.mult),
                  r=[("gx", kk)], w=[("gt", kk)])
            sc.op("act", lambda E: E.activation(out=gs[:, kk, :], in_=gt[:, kk, :], func=AF.Sigmoid,
                                                scale=1.5957691216057308), r=[("gt", kk)], w=[("gs", kk)])
            sc.op("dve", lambda E: E.tensor_tensor(out=dst, in0=gx[:, kk, :], in1=gs[:, kk, :], op=ALU.mult),
                  r=[("gx", kk), ("gs", kk)], w=dst_keys)

        def rstd_ops(src_ap, width, col, stats, junk, key):
            sc.op("dve", lambda E: E.memset(stats[:, col:col + 1], 0.0), w=[(key, "a")])
            sc.op("act", lambda E: E.activation(out=junk[:, 0:width], in_=src_ap, func=AF.Square,
                                                accum_out=stats[:, col:col + 1]), r=[(key, "src")], w=["junk", (key, "a")])
            sc.op("act", lambda E: E.activation(out=stats[:, col + 1:col + 2], in_=stats[:, col:col + 1], func=AF.Sqrt,
                                                bias=epsc[:, 0:1], scale=1.0 / width), r=[(key, "a"), "epsc"], w=[(key, "b")])
            sc.op("dve", lambda E: E.reciprocal(out=stats[:, col + 2:col + 3], in_=stats[:, col + 1:col + 2]),
                  r=[(key, "b")], w=[(key, "r")])

        def mixer_out_phase(li, src_dram, seq):
            with ExitStack() as st:
                def sb(name, shape, dt):
                    uid[0] += 1
                    return st.enter_context(nc.sbuf_tensor(f"{name}_{uid[0]}", shape, dt))
                XB = sb("XB", [128, 4, D], F32)
                XS = sb("XS", [128, 2, D], F32)
                ybt = sb("ybt", [128, 4, D], BF16)
                yT2 = sb("yT", [128, 2, KC, TT], BF16)
                gout = sb("gout", [128, D], F32)
                junk = sb("junk", [128, D], BF16)
                stats = sb("stats", [128, 64], F32)
                sc.op("sp", lambda E: [E.dma_start(out=gout[:], in_=ng_in[li, 3:4, :].partition_broadcast(128))],
                      w=["gout"], dma=("misc",))
                def y_load_T(t0_, hp_):
                    for s in range(4):
                        r0 = t0_ + s * 128
                        sc.op("sp", lambda E, s=s, r0=r0: [E.dma_start(out=ybt[:, s, :], in_=y_d[r0:r0 + 128, :])],
                              r=[("yd", r0 // 128)], w=[("ybt", s)], dma=("ybt", s))
                        transpose_T(lambda kc, s=s: ybt[:, s, kc * 128:(kc + 1) * 128], [("ybt", s)], yT2[:, hp_], s, hp_)
                y_load_T(seq * S, 0)
                for tq in range(S // TT):
                    t0 = seq * S + tq * TT
                    hp = tq % 2
                    yT = yT2[:, hp]
                    sc.op("dve", lambda E: E.memset(stats[:, 0:44], 0.0),
                          w=[("ssp", s, n) for s in range(4) for n in range(4)])
                    def consume(n, s, b):
                        sc.op("dve", lambda E: E.tensor_copy(out=XB[:, s, n * 512:(n + 1) * 512], in_=psum[:, b, :]),
                              r=[("ps", b)], w=[("XB", s)])
                        sc.op("act", lambda E: E.activation(out=junk[:, 0:512], in_=XB[:, s, n * 512:(n + 1) * 512],
                                                            func=AF.Square,
                                                            accum_out=stats[:, 24 + 4 * s + n:25 + 4 * s + n]),
                              r=[("XB", s)], w=["junk", ("ssp", s, n)])
                    chunks = [wo_b[li][c] for c in range(8)]
                    linear_N(lambda kc, s: yT[:, kc, s * 128:(s + 1) * 128], KC, chunks, ("wo", li), 4, 8, consume,
                             lambda s: hT_keys(hp))
                    if tq + 1 < S // TT:
                        y_load_T(t0 + TT, 1 - hp)
                    epilogue(t0, XB, XS, gout, stats, junk, src_dram, 1.0)
            sc.barrier()

        def even_mixer_stage(li, src_dram):
            cast(("wi1", li), wi1_b[li], wi1_in[li])
            cast(("wi2", li), wi2_b[li], wi2_in[li])
            cast(("wo", li), wo_b[li], wo_in[li])
            for seq in range(NB):
                with ExitStack() as st:
                    def sb(name, shape, dt):
                        uid[0] += 1
                        return st.enter_context(nc.sbuf_tensor(f"{name}_{uid[0]}", shape, dt))
                    XB = sb("XB", [128, 2, D], F32)
                    xn = sb("xn", [128, 2, D], BF16)
                    hT = sb("hT", [128, KC, TT], BF16)
                    gin = sb("gin", [128, D], F32)
                    junk = sb("junk", [128, D], BF16)
                    stats = sb("stats", [128, 64], F32)
                    qkf = sb("qkf", [128, 2, 2, TT], F32)
                    rq = sb("rq", [128, 16, TT], BF16)
                    cs = sb("cs", [128, 2, S], F32)
                    rt = sb("rt", [128, 4, TT], F32)
                    vgt = sb("vgt", [128, 4, 4096], BF16)
                    gx = sb("gx", [128, 2, TT], F32)
                    gt = sb("gt", [128, 2, TT], F32)
                    gs = sb("gs", [128, 2, TT], F32)
                    gf = sb("gf", [128, 2, TT], F32)
                    sgain = sb("sgain", [128, 1024], F32)
                    sc.op("sp", lambda E: [E.dma_start(out=gin[:], in_=ng_in[li, 2:3, :].partition_broadcast(128))],
                          w=["gin"], dma=("misc",))
                    sc.op("sp", lambda E: [E.dma_start(out=cs[:], in_=c_rot.rearrange("a p t -> p a t"))],
                          w=["cs"], dma=("misc",))
                    sc.op("sp", lambda E: [E.dma_start(out=sgain[:], in_=sgug_in[li].partition_broadcast(128))],
                          w=["sgain"], dma=("misc",))
                    for tq in range(S // TT):
                        t0 = seq * S + tq * TT
                        ts0 = tq * TT
                        load_norm_T(st, src_dram, t0, None, XB, xn, hT, gin, stats, junk, nxb=2)
                        for c in range(8):
                            slot = ring_load(wi1_b[li][c], 4096, ("wi1", li))
                            pp_ = c % 2
                            for fl in range(2):
                                b = next_bank(0, 4)
                                def mm(E, slot=slot, b=b, fl=fl):
                                    return [E.matmul(psum[:, b, :], lhsT=ring[:, slot, (fl * KC + kc) * 128:(fl * KC + kc + 1) * 128],
                                                     rhs=hT[:, kc, :], start=(kc == 0), stop=(kc == KC - 1)) for kc in range(KC)]
                                sc.op("pe", mm, r=[("ring", slot)] + hT_keys(), w=[("ps", b)])
                                if fl == 0:
                                    sc.op("act", lambda E, b=b, pp_=pp_: E.copy(out=qkf[:, pp_, 0, :], in_=psum[:, b, :]),
                                          r=[("ps", b)], w=[("qkf", pp_, 0)])
                                else:
                                    sc.op("dve", lambda E, b=b, pp_=pp_: E.tensor_copy(out=qkf[:, pp_, 1, :], in_=psum[:, b, :]),
                                          r=[("ps", b)], w=[("qkf", pp_, 1)])
                            A = qkf[:, pp_, 0, :]
                            Bq = qkf[:, pp_, 1, :]
                            co = cs[:, 0, ts0:ts0 + TT]
                            si = cs[:, 1, ts0:ts0 + TT]
                            kA = [("qkf", pp_, 0)]
                            kB = [("qkf", pp_, 1)]
                            sc.op("pool", lambda E, A=A, co=co: E.tensor_tensor(out=rt[:, 0, :], in0=A, in1=co, op=ALU.mult),
                                  r=kA + ["cs"], w=[("rt", 0)])
                            sc.op("pool", lambda E, Bq=Bq, si=si: E.tensor_tensor(out=rt[:, 1, :], in0=Bq, in1=si, op=ALU.mult),
                                  r=kB + ["cs"], w=[("rt", 1)])
                            sc.op("dve", lambda E, c=c: E.tensor_tensor(out=rq[:, 2 * c, :], in0=rt[:, 0, :], in1=rt[:, 1, :],
                                                                       op=ALU.subtract),
                                  r=[("rt", 0), ("rt", 1)], w=[("rq", 2 * c)])
                            sc.op("pool", lambda E, Bq=Bq, co=co: E.tensor_tensor(out=rt[:, 2, :], in0=Bq, in1=co, op=ALU.mult),
                                  r=kB + ["cs"], w=[("rt", 2)])
                            sc.op("pool", lambda E, A=A, si=si: E.tensor_tensor(out=rt[:, 3, :], in0=A, in1=si, op=ALU.mult),
                                  r=kA + ["cs"], w=[("rt", 3)])
                            sc.op("dve", lambda E, c=c: E.tensor_tensor(out=rq[:, 2 * c + 1, :], in0=rt[:, 2, :], in1=rt[:, 3, :],
                                                                       op=ALU.add),
                                  r=[("rt", 2), ("rt", 3)], w=[("rq", 2 * c + 1)])
                        sc.op("sp", lambda E, t0=t0: [E.dma_start(out=qk_d[:, :, t0:t0 + TT].rearrange("c p t -> p c t"),
                                                                  in_=rq[:])],
                              r=[("rq", c) for c in range(16)], w=[("qkd", t0 // TT)], dma=("misc",))
                        kcount = [0]
                        def consume(n, s, b):
                            dst = vgt[:, s, n * 512:(n + 1) * 512]
                            dk = [("vgt", s)]
                            if n < 2:
                                sc.op("act", lambda E: E.copy(out=dst, in_=psum[:, b, :]), r=[("ps", b)], w=dk)
                            elif n < 4:
                                sc.op("act", lambda E: E.activation(out=dst, in_=psum[:, b, :], func=AF.Silu),
                                      r=[("ps", b)], w=dk)
                            elif n < 6:
                                kk = kcount[0] % 2
                                kcount[0] += 1
                                gelu_ops(b, dst, dk, gx, gt, gs, kk)
                            else:
                                kk = kcount[0] % 2
                                kcount[0] += 1
                                gelu_ops(b, gf[:, kk, :], [("gf", kk)], gx, gt, gs, kk)
                                for g2 in range(2):
                                    grp = (n - 6) * 2 + g2
                                    key = ("vsn", kk, g2)
                                    col = 52 + 3 * (2 * kk + g2)
                                    sc.op("dve", lambda E: E.memset(stats[:, col:col + 1], 0.0), w=[(key, "a")])
                                    sc.op("act", lambda E: E.activation(out=junk[:, 0:256], in_=gf[:, kk, g2 * 256:(g2 + 1) * 256],
                                                                        func=AF.Square, accum_out=stats[:, col:col + 1]),
                                          r=[("gf", kk)], w=["junk", (key, "a")])
                                    sc.op("act", lambda E: E.activation(out=stats[:, col + 1:col + 2], in_=stats[:, col:col + 1],
                                                                        func=AF.Sqrt, bias=epsc[:, 0:1], scale=1.0 / 256),
                                          r=[(key, "a"), "epsc"], w=[(key, "b")])
                                    sc.op("dve", lambda E: E.reciprocal(out=stats[:, col + 2:col + 3], in_=stats[:, col + 1:col + 2]),
                                          r=[(key, "b")], w=[(key, "r")])
                                    sc.op("dve", lambda E: E.scalar_tensor_tensor(
                                        out=vgt[:, s, n * 512 + g2 * 256:n * 512 + (g2 + 1) * 256],
                                        in0=gf[:, kk, g2 * 256:(g2 + 1) * 256], scalar=stats[:, col + 2:col + 3],
                                        in1=sgain[:, grp * 256:(grp + 1) * 256], op0=ALU.mult, op1=ALU.mult),
                                        r=[("gf", kk), (key, "r"), "sgain"], w=dk)
                        chunks = [wi2_b[li][c] for c in range(16)]
                        linear_N(lambda kc, s: hT[:, kc, s * 128:(s + 1) * 128], KC, chunks, ("wi2", li), 8, 8, consume,
                                 lambda s: hT_keys())
                        for s in range(4):
                            r0 = t0 + s * 128
                            sc.op("sp", lambda E, s=s, r0=r0: [E.dma_start(out=vg_d[r0:r0 + 128, :], in_=vgt[:, s, :])],
                                  r=[("vgt", s)], w=[("vgd", r0 // 128)], dma=("vgst", s))
                sc.barrier()
                with ExitStack() as st:
                    def sb(name, shape, dt):
                        uid[0] += 1
                        return st.enter_context(nc.sbuf_tensor(f"{name}_{uid[0]}", shape, dt))
                    kTh = sb("kTh", [128, 2, S], BF16)
                    vh = sb("vh", [128, 16, 256], BF16)
                    g2t = sb("g2t", [128, 1024], F32)
                    qTt = sb("qTt", [128, 2, 2, TT], BF16)
                    PT = sb("PT", [128, 3, TT], BF16)
                    osb = sb("osb", [128, 2, 256], F32)
                    sgt = sb("sgt", [128, 2, 4, 256], BF16)
                    yt = sb("yt", [128, 2, 4, 256], BF16)
                    stats = sb("stats", [128, 64], F32)
                    junk = sb("junk", [128, 512], BF16)
                    wsf = sb("wsf", [128, 4, 128], F32)
                    wsb = sb("wsb", [128, 4, 128], BF16)
                    tri = sb("tri", [128, 128], F32)
                    bT = sb("bT", [128, 4], F32)
                    vnt = sb("vnt", [128, 2, 1024], BF16)
                    gut = sb("gut", [128, 2, 1024], BF16)
                    y2 = sb("y2", [128, 2, 1024], BF16)
                    r00 = seq * S
                    pti = [0]
                    for h in range(4):
                        sc.op("sp", lambda E, h=h: [E.dma_start(out=kTh[:], in_=qk_d[8 + 2 * h:10 + 2 * h, :, r00:r00 + S]
                                                                 .rearrange("c p t -> p c t"))],
                              w=["kTh"], dma=("misc",))
                        sc.op("sp", lambda E, h=h: [E.dma_start(out=vh[:], in_=vg_d[r00:r00 + S, h * 256:(h + 1) * 256]
                                                                 .rearrange("(j p) e -> p j e", p=128))],
                              w=["vh"], dma=("misc",))
                        sc.op("sp", lambda E, h=h: [E.dma_start(out=g2t[:], in_=c_g2[h])], w=["g2t"], dma=("misc",))
                        for T in range(4):
                            tp = T % 2
                            t0 = r00 + T * TT
                            sc.op("sp", lambda E, h=h, t0=t0, tp=tp: [E.dma_start(
                                out=qTt[:, tp], in_=qk_d[2 * h:2 * h + 2, :, t0:t0 + TT].rearrange("c p t -> p c t"))],
                                w=[("qTt", tp)], dma=("qTt", tp))
                            sc.op("sp", lambda E, h=h, t0=t0, tp=tp: [E.dma_start(
                                out=sgt[:, tp], in_=vg_d[t0:t0 + TT, 1024 + h * 256:1024 + (h + 1) * 256]
                                .rearrange("(i p) e -> p i e", p=128))], w=[("sgt", tp)], dma=("sgt", tp))
                            nj = 4 * T + 4
                            pend_pv = None
                            for j in range(nj):
                                b = next_bank(0, 4)
                                def smm(E, b=b, j=j, tp=tp):
                                    return [E.matmul(psum[:, b, :], lhsT=kTh[:, c, j * 128:(j + 1) * 128], rhs=qTt[:, tp, c, :],
                                                     start=(c == 0), stop=(c == 1)) for c in range(2)]
                                sc.op("pe", smm, r=["kTh", ("qTt", tp)], w=[("ps", b)])
                                if pend_pv is not None:
                                    pend_pv()
                                    pend_pv = None
                                dl = 4 * T - j
                                if dl >= 1:
                                    off = 512
                                    cc = GAM[h] ** (128.0 * (dl - 1))
                                    jp = -1
                                else:
                                    jp = -dl
                                    off = 384 - 128 * jp
                                    cc = 1.0
                                pk = pti[0] % 3
                                pti[0] += 1
                                sc.op("dve", lambda E, b=b, off=off, cc=cc, pk=pk: E.scalar_tensor_tensor(
                                    out=PT[:, pk, :], in0=psum[:, b, :], scalar=float(cc), in1=g2t[:, off:off + 512],
                                    op0=ALU.mult, op1=ALU.mult), r=[("ps", b), "g2t"], w=[("PT", pk)])
                                subs = [i for i in range(4) if i >= jp]
                                def pv(E, pk=pk, j=j, subs=subs, T=T):
                                    return [E.matmul(psum[:, 4 + i, 0:256], lhsT=PT[:, pk, i * 128:(i + 1) * 128], rhs=vh[:, j, :],
                                                     start=(j == 0), stop=(j == 4 * T + i)) for i in subs]
                                def emit_pv(pv=pv, pk=pk, subs=subs):
                                    sc.op("pe", pv, r=[("PT", pk), "vh"], w=[("ps", 4 + i) for i in subs])
                                pend_pv = emit_pv
                            pend_pv()
                            for i in range(4):
                                ok = i % 2
                                sc.op("dve", lambda E, i=i, ok=ok: E.tensor_copy(out=osb[:, ok, :], in_=psum[:, 4 + i, 0:256]),
                                      r=[("ps", 4 + i)], w=[("osb", ok)])
                                col = 3 * ok
                                key = ("ron", ok)
                                sc.op("dve", lambda E, col=col: E.memset(stats[:, col:col + 1], 0.0), w=[(key, "a")])
                                sc.op("act", lambda E, ok=ok, col=col: E.activation(out=junk[:, 0:256], in_=osb[:, ok, :],
                                                                                  func=AF.Square, accum_out=stats[:, col:col + 1]),
                                      r=[("osb", ok)], w=["junk", (key, "a")])
                                sc.op("act", lambda E, col=col: E.activation(out=stats[:, col + 1:col + 2], in_=stats[:, col:col + 1],
                                                                           func=AF.Sqrt, bias=epsc[:, 0:1], scale=1.0 / 256),
                                      r=[(key, "a"), "epsc"], w=[(key, "b")])
                                sc.op("dve", lambda E, col=col: E.reciprocal(out=stats[:, col + 2:col + 3], in_=stats[:, col + 1:col + 2]),
                                      r=[(key, "b")], w=[(key, "r")])
                                sc.op("dve", lambda E, i=i, ok=ok, col=col, tp=tp: E.scalar_tensor_tensor(
                                    out=yt[:, tp, i, :], in0=osb[:, ok, :], scalar=stats[:, col + 2:col + 3], in1=sgt[:, tp, i, :],
                                    op0=ALU.mult, op1=ALU.mult), r=[("osb", ok), (key, "r"), ("sgt", tp)], w=[("yt", tp)])
                            sc.op("sp", lambda E, h=h, t0=t0, tp=tp: [E.dma_start(
                                out=y_d[t0:t0 + TT, h * 256:(h + 1) * 256].rearrange("(i p) e -> p i e", p=128), in_=yt[:, tp])],
                                r=[("yt", tp)], w=[("yd", t0 // 128 + i) for i in range(4)], dma=("ytst", tp))
                    sc.op("sp", lambda E: [E.dma_start(out=wsf[:], in_=sguw_in[li])], w=["wsf"], dma=("misc",))
                    sc.op("sp", lambda E: [E.dma_start(out=tri[:], in_=c_tri)], w=["tri"], dma=("misc",))
                    sc.op("sp", lambda E: [E.dma_start(out=bT[:], in_=sgub_in[li])], w=["bT"], dma=("misc",))
                    for g in range(4):
                        sc.op("dve", lambda E, g=g: E.tensor_tensor(out=wsb[:, g, :], in0=wsf[:, g, :], in1=tri[:], op=ALU.mult),
                              r=["wsf", "tri"], w=["wsb"])
                    for c in range(16):
                        r0 = r00 + c * 128
                        cp = c % 2
                        sc.op("sp", lambda E, r0=r0, cp=cp: [E.dma_start(out=vnt[:, cp, :], in_=vg_d[r0:r0 + 128, 3072:4096])],
                              w=[("vnt", cp)], dma=("vnt", cp))
                        sc.op("sp", lambda E, r0=r0, cp=cp: [E.dma_start(out=gut[:, cp, :], in_=vg_d[r0:r0 + 128, 2048:3072])],
                              w=[("gut", cp)], dma=("gut", cp))
                        bA = next_bank(0, 4)
                        bB = next_bank(0, 4)
                        def sm(E, cp=cp, bA=bA, bB=bB):
                            out = []
                            for g in range(4):
                                bb = bA if g < 2 else bB
                                out.append(E.matmul(psum[:, bb, (g % 2) * 256:(g % 2 + 1) * 256], lhsT=wsb[:, g, :],
                                                    rhs=vnt[:, cp, g * 256:(g + 1) * 256], start=True, stop=True))
                            return out
                        sc.op("pe", sm, r=["wsb", ("vnt", cp)], w=[("ps", bA), ("ps", bB)])
                        for g in range(4):
                            bb = bA if g < 2 else bB
                            sc.op("dve", lambda E, g=g, bb=bb, cp=cp: E.scalar_tensor_tensor(
                                out=y2[:, cp, g * 256:(g + 1) * 256], in0=psum[:, bb, (g % 2) * 256:(g % 2 + 1) * 256],
                                scalar=bT[:, g:g + 1], in1=gut[:, cp, g * 256:(g + 1) * 256], op0=ALU.add, op1=ALU.mult),
                                r=[("ps", bb), "bT", ("gut", cp)], w=[("y2", cp)])
                        sc.op("sp", lambda E, r0=r0, cp=cp: [E.dma_start(out=y_d[r0:r0 + 128, 1024:2048], in_=y2[:, cp, :])],
                              r=[("y2", cp)], w=[("yd", r0 // 128)], dma=("y2st", cp))
                sc.barrier()
                mixer_out_phase(li, src_dram, seq)

        def odd_mixer_stage(li, src_dram):
            cast(("wi1", li), wi1_b[li], wi1_in[li])
            cast(("wi2", li), wi2_b[li], wi2_in[li])
            cast(("wo", li), wo_b[li], wo_in[li])
            SCL = 128.0 ** -0.5
            for seq in range(NB):
                r00 = seq * S
                with ExitStack() as st:
                    def sb(name, shape, dt):
                        uid[0] += 1
                        return st.enter_context(nc.sbuf_tensor(f"{name}_{uid[0]}", shape, dt))
                    XB = sb("XB", [128, 2, D], F32)
                    xn = sb("xn", [128, 2, D], BF16)
                    hT = sb("hT", [128, KC, TT], BF16)
                    gin = sb("gin", [128, D], F32)
                    junk = sb("junk", [128, D], BF16)
                    stats = sb("stats", [128, 64], F32)
                    qf = sb("qf", [128, 2, TT], F32)
                    qb = sb("qb", [128, 2, TT], BF16)
                    rq = sb("rq", [128, 16, TT], BF16)
                    cs = sb("cs", [128, 2, S], F32)
                    rt = sb("rt", [128, 2, 2, TT], F32)
                    rf = sb("rf", [128, 2, TT], F32)
                    pvt = sb("pvt", [128, 4, D], BF16)
                    permf = sb("permf", [128, 128], F32)
                    permb = sb("permb", [128, 128], BF16)
                    kms = sb("kms", [128, 8, 8], F32)
                    kmb = sb("kmb", [128, 8, 8], BF16)
                    sc.op("sp", lambda E: [E.dma_start(out=gin[:], in_=ng_in[li, 2:3, :].partition_broadcast(128))],
                          w=["gin"], dma=("misc",))
                    sc.op("sp", lambda E: [E.dma_start(out=cs[:], in_=c_rot2.rearrange("a p t -> p a t"))],
                          w=["cs"], dma=("misc",))
                    sc.op("sp", lambda E: [E.dma_start(out=permf[:], in_=c_perm)], w=["permf"], dma=("misc",))
                    sc.op("dve", lambda E: E.tensor_copy(out=permb[:], in_=permf[:]), r=["permf"], w=["permb"])
                    for tq in range(S // TT):
                        t0 = r00 + tq * TT
                        ts0 = tq * TT
                        load_norm_T(st, src_dram, t0, None, XB, xn, hT, gin, stats, junk, nxb=2)
                        for c in range(8):
                            slot = ring_load(wi1_b[li][c], 4096, ("wi1", li))
                            for fl in range(2):
                                fc = 2 * c + fl
                                pq = fc % 2
                                b = next_bank(0, 4)
                                def mm(E, slot=slot, b=b, fl=fl):
                                    return [E.matmul(psum[:, b, :], lhsT=ring[:, slot, (fl * KC + kc) * 128:(fl * KC + kc + 1) * 128],
                                                     rhs=hT[:, kc, :], start=(kc == 0), stop=(kc == KC - 1)) for kc in range(KC)]
                                sc.op("pe", mm, r=[("ring", slot)] + hT_keys(), w=[("ps", b)])
                                sc.op("act", lambda E: E.copy(out=qf[:, pq, :], in_=psum[:, b, :]), r=[("ps", b)], w=[("qf", pq)])
                                sc.op("dve", lambda E: E.tensor_copy(out=qb[:, pq, :], in_=qf[:, pq, :]), r=[("qf", pq)], w=[("qb", pq)])
                                b2 = next_bank(0, 4)
                                sc.op("pe", lambda E: E.matmul(psum[:, b2, :], lhsT=permb[:], rhs=qb[:, pq, :], start=True, stop=True),
                                      r=["permb", ("qb", pq)], w=[("ps", b2)])
                                sc.op("pool", lambda E: E.tensor_tensor(out=rt[:, pq, 0, :], in0=qf[:, pq, :],
                                                                        in1=cs[:, 0, ts0:ts0 + TT], op=ALU.mult),
                                      r=[("qf", pq), "cs"], w=[("rt", pq, 0)])
                                sc.op("dve", lambda E: E.tensor_tensor(out=rt[:, pq, 1, :], in0=psum[:, b2, :],
                                                                       in1=cs[:, 1, ts0:ts0 + TT], op=ALU.mult),
                                      r=[("ps", b2), "cs"], w=[("rt", pq, 1)])
                                if fc < 8:
                                    sc.op("dve", lambda E: E.tensor_tensor(out=rq[:, fc, :], in0=rt[:, pq, 0, :], in1=rt[:, pq, 1, :],
                                                                           op=ALU.add),
                                          r=[("rt", pq, 0), ("rt", pq, 1)], w=[("rq", fc)])
                                else:
                                    sc.op("dve", lambda E: E.tensor_tensor(out=rf[:, pq, :], in0=rt[:, pq, 0, :], in1=rt[:, pq, 1, :],
                                                                           op=ALU.add),
                                          r=[("rt", pq, 0), ("rt", pq, 1)], w=[("rf", pq)])
                                    sc.op("act", lambda E: E.copy(out=rq[:, fc, :], in_=rf[:, pq, :]), r=[("rf", pq)], w=[("rq", fc)])
                                    sc.op("dve", lambda E: E.tensor_reduce(
                                        out=kms[:, fc - 8, 2 * tq:2 * tq + 2], in_=rf[:, pq, :].rearrange("p (b t) -> p b t", b=2),
                                        axis=AX.X, op=ALU.add), r=[("rf", pq)], w=["kms"])
                        sc.op("sp", lambda E, t0=t0: [E.dma_start(out=qk_d[:, :, t0:t0 + TT].rearrange("c p t -> p c t"),
                                                                  in_=rq[:])],
                              r=[("rq", c) for c in range(16)], w=[("qkd", t0 // TT)], dma=("misc",))
                        def consume(n, s, b):
                            dst = pvt[:, s, n * 512:(n + 1) * 512]
                            if (n + s) % 2 == 0:
                                sc.op("act", lambda E: E.copy(out=dst, in_=psum[:, b, :]), r=[("ps", b)], w=[("pvt", s)])
                            else:
                                sc.op("dve", lambda E: E.tensor_copy(out=dst, in_=psum[:, b, :]), r=[("ps", b)], w=[("pvt", s)])
                        chunks = [wi2_b[li][c] for c in range(8)]
                        linear_N(lambda kc, s: hT[:, kc, s * 128:(s + 1) * 128], KC, chunks, ("wi2", li), 4, 8, consume,
                                 lambda s: hT_keys())
                        for s in range(4):
                            r0 = t0 + s * 128
                            sc.op("sp", lambda E, s=s, r0=r0: [E.dma_start(out=pv_d[r0:r0 + 128, :], in_=pvt[:, s, :])],
                                  r=[("pvt", s)], w=[("pvd", r0 // 128)], dma=("pvst", s))
                    sc.op("dve", lambda E: E.tensor_scalar(out=kmb[:], in0=kms[:], scalar1=1.0 / 256.0, scalar2=None, op0=ALU.mult),
                          r=["kms"], w=["kmb"])
                    sc.op("sp", lambda E: [E.dma_start(out=km_d, in_=kmb[:].rearrange("p h n -> p (h n)"))],
                          r=["kmb"], w=["kmd"], dma=("misc",))
                sc.barrier()
                OST = int(os.environ.get("KDBG_OSTOP", "99"))
                if OST == 1:
                    continue
                with ExitStack() as st:
                    def sb(name, shape, dt):
                        uid[0] += 1
                        return st.enter_context(nc.sbuf_tensor(f"{name}_{uid[0]}", shape, dt))
                    kTh = sb("kTh", [128, S], BF16)
                    vhe = sb("vhe", [128, 16, 132], BF16)
                    qTt = sb("qTt", [128, 2, TT], BF16)
                    PT = sb("PT", [128, 3, TT], BF16)
                    biasT = sb("biasT", [128, 2, TT], BF16)
                    CTf = sb("CTf", [128, 4, TT], F32)
                    CT = sb("CT", [128, 4, TT], BF16)
                    indf = sb("indf", [128, 8, 128], F32)
                    ind = sb("ind", [128, 8, 128], BF16)
                    kmb = sb("kmb", [128, 8, 8], BF16)
                    negm = sb("negm", [128, 8, 8], F32)
                    bcon = sb("bcon", [128, 4, 8], F32)
                    g8 = sb("g8", [128, 2, 8], F32)
                    top8 = sb("top8", [128, 2, 8], F32)
                    bf_ = sb("bf_", [128, 2, 8], F32)
                    bb_ = sb("bb_", [128, 2, 8], BF16)
                    rec = sb("rec", [128, 2, 1], F32)
                    ym = sb("ym", [128, 2, 4, 128], BF16)
                    sc.op("sp", lambda E: [E.dma_start(out=CTf[:], in_=c_ct.rearrange("j p t -> p j t"))], w=["CTf"], dma=("misc",))
                    sc.op("dve", lambda E: E.tensor_copy(out=CT[:], in_=CTf[:]), r=["CTf"], w=["CT"])
                    sc.op("sp", lambda E: [E.dma_start(out=indf[0:8], in_=c_ind.rearrange("p (n t) -> p n t", n=8))],
                          w=["indf"], dma=("misc",))
                    sc.op("dve", lambda E: E.tensor_copy(out=ind[0:8], in_=indf[0:8]), r=["indf"], w=["ind"])
                    sc.op("sp", lambda E: [E.dma_start(out=negm[:], in_=c_negm.partition_broadcast(128)
                                                        .rearrange("p o (a b) -> p (o a) b", a=8))], w=["negm"], dma=("misc",))
                    sc.op("sp", lambda E: [E.dma_start(out=bcon[:], in_=c_bcon.partition_broadcast(128)
                                                        .rearrange("p o (a b) -> p (o a) b", a=4))], w=["bcon"], dma=("misc",))
                    sc.op("sp", lambda E: [E.dma_start(out=kmb[:], in_=km_d.rearrange("p (h n) -> p h n", h=8))],
                          r=["kmd"], w=["kmb"], dma=("misc",))
                    sc.op("dve", lambda E: E.memset(vhe[:], 1.0), w=["vhe"])
                    pti = [0]
                    for h in range(8):
                        sc.op("sp", lambda E, h=h: [E.dma_start(out=kTh[:], in_=qk_d[8 + h, :, r00:r00 + S])], w=["kTh"], dma=("misc",))
                        sc.op("sp", lambda E, h=h: [E.dma_start(out=vhe[:, :, 0:128], in_=pv_d[r00:r00 + S, 1024 + h * 128:1024 + (h + 1) * 128]
                                                                 .rearrange("(j p) e -> p j e", p=128))], w=["vhe"], dma=("misc",))
                        for T in range(4):
                            tp = T % 2
                            t0 = r00 + T * TT
                            sc.op("sp", lambda E, h=h, t0=t0, tp=tp: [E.dma_start(out=qTt[:, tp, :], in_=qk_d[h, :, t0:t0 + TT])],
                                  w=[("qTt", tp)], dma=("qTt", tp))
                            for i in range(4):
                                qbk = (4 * T + i) // 2
                                ip = i % 2
                                if qbk <= 3:
                                    sc.op("act", lambda E, ip=ip, qbk=qbk: E.copy(out=bb_[:, ip, :], in_=bcon[:, qbk, :]),
                                          r=["bcon"], w=[("bb", ip)])
                                else:
                                    b = next_bank(0, 4)
                                    sc.op("pe", lambda E, b=b, i=i, tp=tp, h=h: E.matmul(
                                        psum[:, b, 0:8], lhsT=qTt[:, tp, i * 128:(i + 1) * 128], rhs=kmb[:, h, :], start=True, stop=True),
                                        r=[("qTt", tp), "kmb"], w=[("ps", b)])
                                    sc.op("dve", lambda E, b=b, ip=ip, qbk=qbk: E.tensor_tensor(
                                        out=g8[:, ip, :], in0=psum[:, b, 0:8], in1=negm[:, qbk, :], op=ALU.add),
                                        r=[("ps", b), "negm"], w=[("g8", ip)])
                                    sc.op("dve", lambda E, ip=ip: E.max(out=top8[:, ip, :], in_=g8[:, ip, :]),
                                          r=[("g8", ip)], w=[("top8", ip)])
                                    sc.op("dve", lambda E, ip=ip: E.tensor_scalar(
                                        out=bf_[:, ip, :], in0=g8[:, ip, :], scalar1=top8[:, ip, 2:3], scalar2=NEGB,
                                        op0=ALU.is_lt, op1=ALU.mult), r=[("g8", ip), ("top8", ip)], w=[("bf", ip)])
                                    sc.op("dve", lambda E, ip=ip, qbk=qbk: E.memset(bf_[:, ip, qbk:qbk + 1], 0.0), w=[("bf", ip)])
                                    sc.op("act", lambda E, ip=ip: E.copy(out=bb_[:, ip, :], in_=bf_[:, ip, :]),
                                          r=[("bf", ip)], w=[("bb", ip)])
                                b = next_bank(0, 4)
                                sc.op("pe", lambda E, b=b, ip=ip: E.transpose(out=psb[0:8, b, 0:128], in_=bb_[:, ip, :], identity=ident[:]),
                                      r=[("bb", ip), "ident"], w=[("ps", b)])
                                sc.op("act", lambda E, b=b, i=i, tp=tp: E.copy(out=biasT[0:8, tp, i * 128:(i + 1) * 128],
                                                                             in_=psb[0:8, b, 0:128]),
                                      r=[("ps", b)], w=[("biasT", tp)])
                            nj = 4 * T + 4
                            pend_pv = None
                            for j in range(nj):
                                b = next_bank(0, 4)
                                jj = j - 4 * T
                                def smm(E, b=b, j=j, tp=tp, jj=jj):
                                    out = [E.matmul(psum[:, b, :], lhsT=kTh[:, j * 128:(j + 1) * 128], rhs=qTt[:, tp, :],
                                                    start=True, stop=False),
                                           E.matmul(psum[:, b, :], lhsT=ind[0:8, j // 2, :], rhs=biasT[0:8, tp, :],
                                                    start=False, stop=(jj < 0))]
                                    if jj >= 0:
                                        out.append(E.matmul(psum[:, b, :], lhsT=ident[:], rhs=CT[:, jj, :], start=False, stop=True))
                                    return out
                                sc.op("pe", smm, r=["kTh", ("qTt", tp), "ind", ("biasT", tp), "CT", "ident"], w=[("ps", b)])
                                if pend_pv is not None:
                                    pend_pv()
                                    pend_pv = None
                                pk = pti[0] % 3
                                pti[0] += 1
                                sc.op("act", lambda E, b=b, pk=pk: E.activation(out=PT[:, pk, :], in_=psum[:, b, :], func=AF.Exp, scale=SCL),
                                      r=[("ps", b)], w=[("PT", pk)])
                                subs = [i for i in range(4) if i >= jj]
                                def pv(E, pk=pk, j=j, subs=subs, T=T):
                                    return [E.matmul(psum[:, 4 + i, 0:129], lhsT=PT[:, pk, i * 128:(i + 1) * 128], rhs=vhe[:, j, 0:129],
                                                     start=(j == 0), stop=(j == 4 * T + i)) for i in subs]
                                def emit_pv(pv=pv, pk=pk, subs=subs):
                                    sc.op("pe", pv, r=[("PT", pk), "vhe"], w=[("ps", 4 + i) for i in subs])
                                pend_pv = emit_pv
                            pend_pv()
                            for i in range(4):
                                ok = i % 2
                                sc.op("dve", lambda E, i=i, ok=ok: E.reciprocal(out=rec[:, ok, :], in_=psum[:, 4 + i, 128:129]),
                                      r=[("ps", 4 + i)], w=[("rec", ok)])
                                sc.op("dve", lambda E, i=i, ok=ok, tp=tp: E.tensor_scalar(
                                    out=ym[:, tp, i, :], in0=psum[:, 4 + i, 0:128], scalar1=rec[:, ok, :], scalar2=None, op0=ALU.mult),
                                    r=[("ps", 4 + i), ("rec", ok)], w=[("ym", tp)])
                            sc.op("sp", lambda E, h=h, t0=t0, tp=tp: [E.dma_start(
                                out=y_d[t0:t0 + TT, 1024 + h * 128:1024 + (h + 1) * 128].rearrange("(i p) e -> p i e", p=128),
                                in_=ym[:, tp])], r=[("ym", tp)], w=[("yd", t0 // 128 + i) for i in range(4)], dma=("ymst", tp))
                sc.barrier()
                if OST == 2:
                    continue
                with ExitStack() as st:
                    def sb(name, shape, dt):
                        uid[0] += 1
                        return st.enter_context(nc.sbuf_tensor(f"{name}_{uid[0]}", shape, dt))
                    apf = sb("apf", [128, 12, 128], F32)
                    apb = sb("apb", [128, 12, 128], BF16)
                    wpf = sb("wpf", [128, 8, 256], F32)
                    wpb = sb("wpb", [128, 8, 256], BF16)
                    psc = sb("psc", [128, 1024], F32)
                    pzt = sb("pzt", [128, 3, 1024], BF16)
                    yTb = sb("yTb", [128, 2, 8, 128], BF16)
                    y1 = sb("y1", [128, 2, 1024], BF16)
                    sc.op("sp", lambda E: [E.dma_start(out=apf[:], in_=c_pool.rearrange("a p t -> p a t"))], w=["apf"], dma=("misc",))
                    sc.op("dve", lambda E: E.tensor_copy(out=apb[:], in_=apf[:]), r=["apf"], w=["apb"])
                    sc.op("sp", lambda E: [E.dma_start(out=wpf[:], in_=poolw_in[li])], w=["wpf"], dma=("misc",))
                    sc.op("dve", lambda E: E.tensor_copy(out=wpb[:], in_=wpf[:]), r=["wpf"], w=["wpb"])
                    sc.op("sp", lambda E: [E.dma_start(out=psc[:], in_=pools_in[li].partition_broadcast(128))], w=["psc"], dma=("misc",))
                    for c in range(16):
                        r0 = r00 + c * 128
                        cp = c % 3
                        pp_ = (c - 1) % 3
                        c2 = c % 2
                        sc.op("sp", lambda E, r0=r0, cp=cp: [E.dma_start(out=pzt[:, cp, :], in_=pv_d[r0:r0 + 128, 0:1024])],
                              w=[("pzt", cp)], dma=("pzt", cp))
                        bA = next_bank(0, 8)
                        bB = next_bank(0, 8)
                        def pm(E, c=c, cp=cp, pp_=pp_, bA=bA, bB=bB):
                            out = []
                            for gi in range(4):
                                for cc in range(2):
                                    idx = gi * 2 + cc
                                    bb = bA if idx < 4 else bB
                                    dst = psum[:, bb, (idx % 4) * 128:(idx % 4 + 1) * 128]
                                    lcur = pzt[:, cp, gi * 256 + cc * 128:gi * 256 + (cc + 1) * 128]
                                    if c == 0:
                                        out.append(E.matmul(dst, lhsT=lcur, rhs=apb[:, 4 + gi, :], start=True, stop=True))
                                    else:
                                        lprev = pzt[:, pp_, gi * 256 + cc * 128:gi * 256 + (cc + 1) * 128]
                                        out.append(E.matmul(dst, lhsT=lcur, rhs=apb[:, gi, :], start=True, stop=False))
                                        out.append(E.matmul(dst, lhsT=lprev, rhs=apb[:, 8 + gi, :], start=False, stop=True))
                            return out
                        sc.op("pe", pm, r=[("pzt", cp), ("pzt", pp_), "apb"], w=[("ps", bA), ("ps", bB)])
                        sc.op("act", lambda E, bA=bA, c2=c2: E.copy(out=yTb[:, c2, 0:4, :], in_=psum[:, bA, :].rearrange("p (a t) -> p a t", a=4)),
                              r=[("ps", bA)], w=[("yTb", c2, 0)])
                        sc.op("dve", lambda E, bB=bB, c2=c2: E.tensor_copy(out=yTb[:, c2, 4:8, :], in_=psum[:, bB, :].rearrange("p (a t) -> p a t", a=4)),
                              r=[("ps", bB)], w=[("yTb", c2, 1)])
                        bC = next_bank(0, 8)
                        bD = next_bank(0, 8)
                        def pl(E, c2=c2, bC=bC, bD=bD):
                            out = []
                            for gi in range(4):
                                bb = bC if gi < 2 else bD
                                for cc in range(2):
                                    out.append(E.matmul(psum[:, bb, (gi % 2) * 256:(gi % 2 + 1) * 256], lhsT=yTb[:, c2, gi * 2 + cc, :],
                                                        rhs=wpb[:, gi * 2 + cc, :], start=(cc == 0), stop=(cc == 1)))
                            return out
                        sc.op("pe", pl, r=[("yTb", c2, 0), ("yTb", c2, 1), "wpb"], w=[("ps", bC), ("ps", bD)])
                        for half, bb in ((0, bC), (1, bD)):
                            sc.op("dve", lambda E, half=half, bb=bb, c2=c2: E.tensor_tensor(
                                out=y1[:, c2, half * 512:(half + 1) * 512], in0=psum[:, bb, :], in1=psc[:, half * 512:(half + 1) * 512],
                                op=ALU.mult), r=[("ps", bb), "psc"], w=[("y1", c2)])
                        sc.op("sp", lambda E, r0=r0, c2=c2: [E.dma_start(out=y_d[r0:r0 + 128, 0:1024], in_=y1[:, c2, :])],
                              r=[("y1", c2)], w=[("yd", r0 // 128)], dma=("y1st", c2))
                sc.barrier()
                mixer_out_phase(li, src_dram, seq)

        for li in layers:
            cast(("gu", li * 2), gu_b[li * 2], gu_in[li * 2])
            cast(("dn", li * 2), wd_b[li * 2], wd_in[li * 2])
            cast(("wi1", li), wi1_b[li], wi1_in[li])
            cast(("wi2", li), wi2_b[li], wi2_in[li])
            cast(("wo", li), wo_b[li], wo_in[li])
            cast(("gu", li * 2 + 1), gu_b[li * 2 + 1], gu_in[li * 2 + 1])
            cast(("dn", li * 2 + 1), wd_b[li * 2 + 1], wd_in[li * 2 + 1])
            cast(("pg", li), pg_b[li], pg_in[li])
            cast(("pp", li), pp_b[li], pp_in[li])
        first = True
        for idx, li in enumerate(layers):
            nst = n_stage if idx == len(layers) - 1 else 4
            src = x_in if first else y_out
            ffn_stage(li, 0, src)
            first = False
            if nst >= 2:
                if li % 2 == 0:
                    even_mixer_stage(li, y_out)
                else:
                    odd_mixer_stage(li, y_out)
            if nst >= 3:
                ffn_stage(li, 1, y_out)
            if nst >= 4:
                ple_stage(li, y_out)
        sc.barrier(engines=("sp",))
        print("ops", sc.nops, "sems", len(sc.sem))
    return nc


def _p1_chunks(W):
    K, F = W.shape
    a = W.reshape(K // 128, 128, F // 256, 2, 128)
    a = a.transpose(2, 1, 3, 0, 4)
    return np.ascontiguousarray(a).reshape(F // 256, 128, -1)


def _p2_chunks(W, kper):
    K, N = W.shape
    ng = K // 128 // kper
    a = W.reshape(ng, kper, 128, N // 512, 512)
    a = a.transpose(3, 0, 2, 1, 4)
    return np.ascontiguousarray(a).reshape(N // 512 * ng, 128, kper * 512)


def _gu_chunks(Wg, Wu):
    K, F = Wg.shape
    g = Wg.reshape(K // 128, 128, F // 128, 128).transpose(2, 1, 0, 3)
    u = Wu.reshape(K // 128, 128, F // 128, 128).transpose(2, 1, 0, 3)
    a = np.stack([g, u], axis=2)
    return np.ascontiguousarray(a).reshape(F // 128, 128, -1)


def prep_weights(inp, layers):
    out = {}
    for li in layers:
        for j in range(2):
            f = li * 2 + j
            out[f"w_gu{f}"] = _gu_chunks(inp["w_ffn_gate"][li, j], inp["w_ffn_up"][li, j])
            out[f"w_dn{f}"] = _p2_chunks(inp["w_ffn_down"][li, j], 11)
        out[f"w_pg{li}"] = _p2_chunks(inp["w_ple_gate"][li], 8)
        out[f"w_pp{li}"] = np.ascontiguousarray(
            inp["w_ple_proj"][li].reshape(2, 128, D).transpose(1, 0, 2)).reshape(128, 2 * D)
        jj = li // 2
        if li % 2 == 0:
            wi = inp["w_in_even"][jj]
            out[f"w_i1_{li}"] = _p1_chunks(wi[:, 0:2048])
            out[f"w_i2_{li}"] = _p2_chunks(wi[:, 2048:6144], 8)
            out[f"w_o_{li}"] = _p2_chunks(inp["w_out_even"][jj], 8)
            out[f"sgu_wT_{li}"] = np.ascontiguousarray(inp["sgu_w"][jj].transpose(2, 0, 1))
            out[f"sgu_bT_{li}"] = np.ascontiguousarray(inp["sgu_b"][jj].T)
            out[f"sgu_g_{li}"] = np.ascontiguousarray(inp["sgu_gain"][jj]).reshape(1, 1024)
        else:
            wi = inp["w_in_odd"][jj]
            out[f"w_i1_{li}"] = _p1_chunks(wi[:, 1024:3072])
            out[f"w_i2_{li}"] = _p2_chunks(np.concatenate([wi[:, 0:1024], wi[:, 3072:4096]], axis=1), 8)
            out[f"w_o_{li}"] = _p2_chunks(inp["w_out_odd"][jj], 8)
            out[f"pool_w_{li}"] = np.ascontiguousarray(
                inp["pool_w"][jj].reshape(4, 2, 128, 256).transpose(2, 0, 1, 3)).reshape(128, 8, 256)
            out[f"pool_s_{li}"] = np.ascontiguousarray(inp["pool_scale"][jj]).reshape(1, 1024)
    out.update(_constants())
    return out


def _constants():
    c = {}
    j = np.arange(128, dtype=np.float64)
    t = np.arange(S, dtype=np.float64)
    inv = 1.0 / np.power(10000.0, j / 128.0)
    ang = (t[None, :].astype(np.float32) * inv[:, None].astype(np.float32)).astype(np.float32)
    c["c_rot"] = np.stack([np.cos(ang), np.sin(ang)]).astype(np.float32)
    g2 = np.zeros((4, 128, 1024), np.float64)
    sl = np.arange(128)[:, None]
    w = np.arange(1024)[None, :]
    e = w - 384 - sl
    for h in range(4):
        gam = 1.0 - 2.0 ** (-5.0 - h)
        g2[h] = np.where(e >= 0, np.power(gam, np.maximum(e, 0)), 0.0) / 16.0
    c["c_g2"] = g2.astype(np.float32)
    c["c_tri"] = (np.arange(128)[None, :] >= np.arange(128)[:, None]).astype(np.float32)
    dd = np.arange(128)
    inv2 = (1.0 / np.power(np.float32(500000.0), (np.arange(16, dtype=np.float32) / 16.0))).astype(np.float32)
    ang2 = t[None, :].astype(np.float32) * inv2[dd % 16][:, None]
    C = np.where(dd[:, None] < 32, np.cos(ang2), 1.0)
    Sn = np.where(dd[:, None] < 32, np.sin(ang2), 0.0)
    c["c_rot2"] = np.stack([C, Sn]).astype(np.float32)
    perm = np.zeros((128, 128), np.float32)
    for d_ in range(16):
        perm[d_ + 16, d_] = -1.0
        perm[d_, d_ + 16] = 1.0
    c["c_perm"] = perm
    ct = np.zeros((4, 128, TT), np.float32)
    sl_ = np.arange(128)[:, None]
    tl_ = np.arange(TT)[None, :]
    for jj in range(4):
        ct[jj] = np.where((tl_ // 256 == jj // 2) & (128 * jj + sl_ > tl_), NEGB, 0.0)
    c["c_ct"] = ct
    ind = np.zeros((8, 8, 128), np.float32)
    for n in range(8):
        ind[n, n, :] = 1.0
    c["c_ind"] = ind.reshape(8, 8 * 128)
    negm = np.zeros((8, 8), np.float32)
    for qb in range(8):
        negm[qb, qb:] = -1e30
    c["c_negm"] = negm.reshape(1, 64)
    bcon = np.zeros((4, 8), np.float32)
    for qb in range(4):
        bcon[qb, qb + 1:] = NEGB
    c["c_bcon"] = bcon.reshape(1, 32)
    pool = np.zeros((12, 128, 128), np.float64)
    ss_ = np.arange(128)[:, None]
    tt_ = np.arange(128)[None, :]
    for gi, w_ in enumerate((2, 4, 8, 16)):
        band = (ss_ <= tt_) & (ss_ >= tt_ - w_ + 1)
        pool[gi] = np.where(band, 1.0 / w_, 0.0) - (ss_ == tt_)
        cnt = np.minimum(tt_ + 1, w_)
        pool[4 + gi] = np.where(band, 1.0 / cnt, 0.0) - (ss_ == tt_)
        pool[8 + gi] = np.where(ss_ >= tt_ + 129 - w_, 1.0 / w_, 0.0)
    c["c_pool"] = pool.astype(np.float32)
    return c


def run(inputs, n_cores, NB, layers, n_stage=4, trace=False):
    inp = {k: np.asarray(v) for k, v in inputs.items()}
    nc = build_program(NB, layers, n_stage)
    wts = prep_weights(inp, layers)
    in_maps = []
    for c in range(n_cores):
        m = dict(wts)
        m["x"] = np.ascontiguousarray(inp["x"][c * NB:(c + 1) * NB]).reshape(NB * S, D)
        m["p"] = np.ascontiguousarray(inp["p"][:, c * NB:(c + 1) * NB]).reshape(4, NB * S, 256)
        m["norm_gains"] = inp["norm_gains"]
        in_maps.append(m)
    res = run_bass_kernel_spmd(nc, in_maps, core_ids=list(range(n_cores)), trace=trace)
    out = np.stack([r["out"].reshape(NB, S, D) for r in res.results]).reshape(n_cores * NB, S, D)
    return out, res


def kernel(**inputs):
    out, _ = run(inputs, 8, 2, [0, 1, 2, 3])
    return out.astype(np.float32)
```

```python
import math
from contextlib import ExitStack
import numpy as np
import concourse.bass as bass
import concourse.mybir as mybir
from concourse.bass_utils import run_bass_kernel_spmd

F32 = mybir.dt.float32
BF16 = mybir.dt.bfloat16
AF = mybir.ActivationFunctionType
ALU = mybir.AluOpType
AX = mybir.AxisListType

D = 2048
DFF = 5632
S = 2048
TT = 512
KC = 16
FC = 44
EPS = 1e-6
NEGB = -30000.0
RING = 4
RSZ = 5632
import os
DBG_STOP = int(os.environ.get("KDBG_STOP", "99"))


class Sched:
    def __init__(self, nc, es):
        self.nc = nc
        self.es = es
        self.E = {"pe": nc.tensor, "act": nc.scalar, "dve": nc.vector, "pool": nc.gpsimd, "sp": nc.sync}
        self.sem = {}
        self.cnt = {}
        self.last_w = {}
        self.rd = {}
        self.seen = {k: {} for k in self.E}
        self.last_sig = {}
        self.nops = 0

    def _sem(self, key):
        if key not in self.sem:
            self.sem[key] = self.es.enter_context(self.nc.semaphore("s_" + "_".join(str(k) for k in key)))
            self.cnt[key] = 0
        return self.sem[key]

    def _wait(self, eng, key, val):
        if self.seen[eng].get(key, 0) >= val:
            return
        self.E[eng].wait_ge(self._sem(key), val)
        self.seen[eng][key] = val

    def op(self, eng, fn, r=(), w=(), dma=None, ndma=1):
        if dma is not None and dma[0] in ("misc", "cast") and ("dma", dma) in self.cnt:
            self._wait(eng, ("dma", dma), self.cnt[("dma", dma)])
        deps = {}
        for k in r:
            if k in self.last_w:
                sk, v = self.last_w[k]
                deps[sk] = max(deps.get(sk, 0), v)
        for k in w:
            if k in self.last_w:
                sk, v = self.last_w[k]
                deps[sk] = max(deps.get(sk, 0), v)
            for sk, v in self.rd.get(k, {}).items():
                deps[sk] = max(deps.get(sk, 0), v)
        mykey = ("dma", dma) if dma is not None else ("eng", eng)
        for sk, v in deps.items():
            if sk == ("eng", "pe") and eng == "pe" and dma is None:
                continue
            self._wait(eng, sk, v)
        res = fn(self.E[eng])
        sem = self._sem(mykey)
        if dma is not None:
            assert len(res) == ndma
            for ins in res:
                ins.then_inc(sem, 16)
            self.cnt[mykey] += 16 * ndma
        else:
            last = res[-1] if isinstance(res, (list, tuple)) else res
            last.then_inc(sem, 1)
            self.cnt[mykey] += 1
        val = self.cnt[mykey]
        self.last_sig[mykey] = val
        for k in r:
            d = self.rd.setdefault(k, {})
            d[mykey] = val
        for k in w:
            self.last_w[k] = (mykey, val)
            self.rd[k] = {}
        self.nops += 1

    def barrier(self, engines=("pe", "act", "dve", "pool", "sp")):
        for eng in engines:
            for sk, v in self.last_sig.items():
                self._wait(eng, sk, v)


def build_program(NB, layers, n_stage=4, debug_out=False):
    NT = NB * S
    NTT = NT // TT
    import os
    if os.environ.get("KDBG_NTT"):
        NTT = int(os.environ["KDBG_NTT"])
    nc = bass.Bass("TRN2", target_bir_lowering=False)
    es = ExitStack()
    with es:
        def din(name, shape, dt=F32):
            return nc.dram_tensor(name, list(shape), dt, kind="ExternalInput").ap()
        x_in = din("x", [NT, D])
        p_in = din("p", [4, NT, 256])
        ng_in = din("norm_gains", [4, 8, D])
        y_out = nc.dram_tensor("out", [NT, D], F32, kind="ExternalOutput").ap()
        def dbf(name, shape):
            return nc.dram_tensor(name, list(shape), BF16).ap()
        gu_in, wd_in, gu_b, wd_b, pg_in, pp_in, pg_b, pp_b = {}, {}, {}, {}, {}, {}, {}, {}
        wi1_in, wi2_in, wo_in, wi1_b, wi2_b, wo_b = {}, {}, {}, {}, {}, {}
        for li in layers:
            for j in range(2):
                f = li * 2 + j
                gu_in[f] = din(f"w_gu{f}", [FC, 128, 4096])
                wd_in[f] = din(f"w_dn{f}", [16, 128, RSZ])
                gu_b[f] = dbf(f"b_gu{f}", [FC, 128, 4096])
                wd_b[f] = dbf(f"b_dn{f}", [16, 128, RSZ])
            pg_in[li] = din(f"w_pg{li}", [8, 128, 4096])
            pp_in[li] = din(f"w_pp{li}", [128, 4096])
            pg_b[li] = dbf(f"b_pg{li}", [8, 128, 4096])
            pp_b[li] = dbf(f"b_pp{li}", [128, 4096])

        ev_layers = [li for li in layers if li % 2 == 0]
        wi1_in, wi2_in, wo_in, wi1_b, wi2_b, wo_b = {}, {}, {}, {}, {}, {}
        sguw_in, sgub_in, sgug_in = {}, {}, {}
        for li in layers:
            n1, n2 = (8, 16) if li % 2 == 0 else (8, 8)
            wi1_in[li] = din(f"w_i1_{li}", [n1, 128, 4096])
            wi2_in[li] = din(f"w_i2_{li}", [n2, 128, 4096])
            wo_in[li] = din(f"w_o_{li}", [8, 128, 4096])
            wi1_b[li] = dbf(f"b_i1_{li}", [n1, 128, 4096])
            wi2_b[li] = dbf(f"b_i2_{li}", [n2, 128, 4096])
            wo_b[li] = dbf(f"b_o_{li}", [8, 128, 4096])
            if li % 2 == 0:
                sguw_in[li] = din(f"sgu_wT_{li}", [128, 4, 128])
                sgub_in[li] = din(f"sgu_bT_{li}", [128, 4])
                sgug_in[li] = din(f"sgu_g_{li}", [1, 1024])
        poolw_in, pools_in = {}, {}
        for li in layers:
            if li % 2 == 1:
                poolw_in[li] = din(f"pool_w_{li}", [128, 8, 256])
                pools_in[li] = din(f"pool_s_{li}", [1, 1024])
        c_rot2 = din("c_rot2", [2, 128, S])
        c_perm = din("c_perm", [128, 128])
        c_ct = din("c_ct", [4, 128, TT])
        c_ind = din("c_ind", [8, 8 * 128])
        c_negm = din("c_negm", [1, 64])
        c_bcon = din("c_bcon", [1, 32])
        c_pool = din("c_pool", [12, 128, 128])
        pv_d = dbf("pv_d", [NT, D])
        km_d = dbf("km_d", [128, 64])
        c_rot = din("c_rot", [2, 128, S])
        c_g2 = din("c_g2", [4, 128, 1024])
        c_tri = din("c_tri", [128, 128])
        qk_d = dbf("qk_d", [16, 128, NT])
        vg_d = dbf("vg_d", [NT, 4096])
        y_d = dbf("y_d", [NT, D])
        sc = Sched(nc, es)
        ring = es.enter_context(nc.sbuf_tensor("ring", [128, RING, RSZ], BF16))
        ident = es.enter_context(nc.sbuf_tensor("ident", [128, 128], BF16))
        identf = es.enter_context(nc.sbuf_tensor("identf", [128, 128], F32))
        psum = es.enter_context(nc.psum_tensor("psum", [128, 8, 512], F32))
        psb = psum[:].bitcast(BF16)

        epsc = es.enter_context(nc.sbuf_tensor("epsc", [128, 4], F32))
        sc.op("dve", lambda E: E.memset(epsc[:], EPS), w=["epsc"])
        sc.op("pool", lambda E: E.memset(identf[:], 0.0), w=["identf"])
        sc.op("pool", lambda E: E.affine_select(out=identf[:], in_=identf[:], pattern=[[-1, 128]],
                                                compare_op=ALU.not_equal, fill=1.0, base=0, channel_multiplier=1),
              w=["identf"])
        sc.op("dve", lambda E: E.tensor_copy(out=ident[:], in_=identf[:]), r=["identf"], w=["ident"])

        cast_done = set()
        cast_n = [0]
        def cast(key, dst, src):
            if key in cast_done:
                return
            cast_done.add(key)
            n = dst.shape[0] if len(dst.shape) == 3 else 1
            def f(E):
                if n == 1:
                    return [E.dma_start(out=dst, in_=src)]
                return [E.dma_start(out=dst[c], in_=src[c]) for c in range(n)]
            cast_n[0] += 1
            sc.op("pool", f, w=[("wb", key)], dma=("cast", cast_n[0] % 2), ndma=n)

        ring_state = {"n": 0}
        def ring_load(src_ap, nel, wkey):
            i = ring_state["n"] % RING
            ring_state["n"] += 1
            sc.op("sp", lambda E: [E.dma_start(out=ring[:, i, 0:nel], in_=src_ap)],
                  r=[("wb", wkey)], w=[("ring", i)], dma=("ring", i))
            return i

        uid = [0]
        bank_state = {"n": 0}
        def next_bank(lo=0, hi=8):
            b = lo + bank_state["n"] % (hi - lo)
            bank_state["n"] += 1
            return b

        def transpose_T(src_fn, src_keys, hT, s, hp=0):
            for half in range(2):
                b = next_bank(0, 4)
                def tr(E, half=half, b=b):
                    out = []
                    for q in range(8):
                        kc = half * 8 + q
                        out.append(E.transpose(out=psb[:, b, q * 128:(q + 1) * 128], in_=src_fn(kc), identity=ident[:]))
                    return out
                sc.op("pe", tr, r=list(src_keys) + ["ident"], w=[("ps", b)])
                eng = "act" if half == 0 else "dve"
                def ev(E, half=half, b=b, eng=eng):
                    src = psb[:, b, :].rearrange("p (q t) -> p q t", q=8)
                    dst = hT[:, half * 8:(half + 1) * 8, s * 128:(s + 1) * 128]
                    if eng == "act":
                        return E.copy(out=dst, in_=src)
                    return E.tensor_copy(out=dst, in_=src)
                sc.op(eng, ev, r=[("ps", b)], w=[("hT", hp, s, half)])

        def norm_parts(src_dram, t0, XB, xn, hT, gin, stats, junk, nxb=4, hp=0, ioq="sp"):
            def zero():
                sc.op("dve", lambda E: E.memset(stats[:, 0:4], 0.0), w=[("ss", s) for s in range(4)])
            parts = []
            for s in range(4):
                def norm_s(s=s):
                    r0 = t0 + s * 128
                    xs = s % nxb
                    j = s % 2
                    sc.op(ioq, lambda E: [E.dma_start(out=XB[:, xs, :], in_=src_dram[r0:r0 + 128, :])],
                          r=[("xd", r0 // 128)], w=[("XB", xs)], dma=("XB", xs))
                    sc.op("act", lambda E: E.activation(out=junk[:], in_=XB[:, xs, :], func=AF.Square,
                                                        accum_out=stats[:, s:s + 1]),
                          r=[("XB", xs)], w=["junk", ("ss", s)])
                    sc.op("act", lambda E: E.activation(out=stats[:, 8 + s:9 + s], in_=stats[:, s:s + 1], func=AF.Sqrt,
                                                        bias=epsc[:, 0:1], scale=1.0 / D),
                          r=[("ss", s), "epsc"], w=[("rs_a", s)])
                    sc.op("dve", lambda E: E.reciprocal(out=stats[:, 16 + s:17 + s], in_=stats[:, 8 + s:9 + s]),
                          r=[("rs_a", s)], w=[("rs", s)])
                    sc.op("dve", lambda E: E.scalar_tensor_tensor(
                        out=xn[:, j, :], in0=XB[:, xs, :], scalar=stats[:, 16 + s:17 + s], in1=gin[:],
                        op0=ALU.mult, op1=ALU.mult),
                        r=[("XB", xs), ("rs", s), "gin"], w=[("xn", j)])
                def tr_s(s=s):
                    j = s % 2
                    transpose_T(lambda kc: xn[:, j, kc * 128:(kc + 1) * 128], [("xn", j)], hT, s, hp)
                parts.append((norm_s, tr_s))
            return zero, parts

        def load_norm_T(st, src_dram, t0, gain_row, XB, xn, hT, gin, stats, junk, nxb=4, hp=0, ioq="sp"):
            zero, parts = norm_parts(src_dram, t0, XB, xn, hT, gin, stats, junk, nxb, hp, ioq)
            zero()
            for nfn, tfn in parts:
                nfn()
                tfn()

        def hT_keys(hp=0):
            return [("hT", hp, s, h) for s in range(4) for h in range(2)]

        def epilogue_parts(t0, XB, XS, gout, stats, junk, src_dram, coef, ioq="sp"):
            parts = []
            for s in range(4):
                def part(s=s):
                    r0 = t0 + s * 128
                    j = s % 2
                    sc.op(ioq, lambda E: [E.dma_start(out=XS[:, j, :], in_=src_dram[r0:r0 + 128, :])],
                          r=[("xd", r0 // 128)], w=[("XS", j)], dma=("XS", j))
                    sc.op("dve", lambda E: E.tensor_reduce(out=stats[:, 40 + s:41 + s],
                                                           in_=stats[:, 24 + 4 * s:28 + 4 * s],
                                                           axis=AX.X, op=ALU.add),
                          r=[("ssp", s, n) for n in range(4)], w=[("sso", s)])
                    sc.op("act", lambda E: E.activation(out=stats[:, 44 + s:45 + s], in_=stats[:, 40 + s:41 + s],
                                                        func=AF.Sqrt, bias=epsc[:, 0:1], scale=1.0 / D),
                          r=[("sso", s), "epsc"], w=[("rso_a", s)])
                    sc.op("dve", lambda E: E.reciprocal(out=stats[:, 48 + s:49 + s], in_=stats[:, 44 + s:45 + s]),
                          r=[("rso_a", s)], w=[("rso", s)])
                    sc.op("dve", lambda E: E.scalar_tensor_tensor(
                        out=XB[:, s, :], in0=XB[:, s, :], scalar=stats[:, 48 + s:49 + s], in1=gout[:],
                        op0=ALU.mult, op1=ALU.mult),
                        r=[("rso", s), "gout"], w=[("XB", s)])
                    sc.op("dve", lambda E: E.scalar_tensor_tensor(
                        out=XS[:, j, :], in0=XB[:, s, :], scalar=float(coef), in1=XS[:, j, :],
                        op0=ALU.mult, op1=ALU.add),
                        r=[("XB", s)], w=[("XS", j)])
                    sc.op(ioq, lambda E: [E.dma_start(out=y_out[r0:r0 + 128, :], in_=XS[:, j, :])],
                          r=[("XS", j)], w=[("xd", r0 // 128)], dma=("XSst", j))
                parts.append(part)
            return parts

        def epilogue(t0, XB, XS, gout, stats, junk, src_dram, coef, ioq="sp"):
            for p_ in epilogue_parts(t0, XB, XS, gout, stats, junk, src_dram, coef, ioq):
                p_()

        def linear_N(hT_ap_fn, nk, src_chunks, wkey, n_cols_chunks, kper, consume, in_keys):
            ng = nk // kper
            for n in range(n_cols_chunks):
                banks = [next_bank() for _ in range(4)]
                for g in range(ng):
                    slot = ring_load(src_chunks[n * ng + g], kper * 512, wkey)
                    for s in range(4):
                        def mm(E, s=s, g=g, slot=slot, b=banks[s]):
                            out = []
                            for q in range(kper):
                                kc = g * kper + q
                                out.append(E.matmul(psum[:, b, :], lhsT=hT_ap_fn(kc, s),
                                                    rhs=ring[:, slot, q * 512:(q + 1) * 512],
                                                    start=(kc == 0), stop=(kc == nk - 1)))
                            return out
                        sc.op("pe", mm, r=[("ring", slot)] + in_keys(s), w=[("ps", banks[s])])
                for s in range(4):
                    consume(n, s, banks[s])

        def ffn_stage(li, j, src_dram):
            f = li * 2 + j
            cast(("gu", f), gu_b[f], gu_in[f])
            cast(("dn", f), wd_b[f], wd_in[f])
            with ExitStack() as st:
                def sb(name, shape, dt):
                    uid[0] += 1
                    return st.enter_context(nc.sbuf_tensor(f"{name}_{uid[0]}", shape, dt))
                XB = sb("XB", [128, 4, D], F32)
                XS = sb("XS", [128, 2, D], F32)
                xn = sb("xn", [128, 2, D], BF16)
                hT2 = sb("hT", [128, 2, KC, TT], BF16)
                act = sb("act", [128, FC, TT], BF16)
                gin = sb("gin", [128, D], F32)
                gout = sb("gout", [128, D], F32)
                sg = sb("sg", [128, 2, TT], F32)
                junk = sb("junk", [128, D], BF16)
                stats = sb("stats", [128, 64], F32)
                gi = 0 if j == 0 else 4
                sc.op("sp", lambda E: [E.dma_start(out=gin[:], in_=ng_in[li, gi:gi + 1, :].partition_broadcast(128))],
                      w=["gin"], dma=("misc",))
                sc.op("sp", lambda E: [E.dma_start(out=gout[:], in_=ng_in[li, gi + 1:gi + 2, :].partition_broadcast(128))],
                      w=["gout"], dma=("misc",))
                load_norm_T(st, src_dram, 0, None, XB, xn, hT2[:, 0], gin, stats, junk, hp=0, ioq="pool")
                pend = None
                for tt in range(NTT):
                    t0 = tt * TT
                    hp = tt % 2
                    hT = hT2[:, hp]
                    pre = None
                    if tt + 1 < NTT:
                        pre = norm_parts(src_dram, t0 + TT, XB, xn, hT2[:, 1 - hp], gin, stats, junk, 4, 1 - hp, "pool")
                    sched = {}
                    if pend is not None:
                        for s_ in range(4):
                            sched.setdefault(1 + 2 * s_, []).append(pend[s_])
                    if pre is not None:
                        sched.setdefault(12, []).extend([pre[0], pre[1][0][0], pre[1][1][0]])
                        sched.setdefault(24, []).extend([pre[1][0][1], pre[1][1][1]])
                        sched.setdefault(26, []).extend([pre[1][2][0], pre[1][3][0]])
                    for fc in range(FC):
                        for fn_ in sched.get(fc, []):
                            fn_()
                        slot = ring_load(gu_b[f][fc], 4096, ("gu", f))
                        bg = next_bank()
                        bu = next_bank()
                        def mmgu(E, slot=slot, bg=bg, bu=bu):
                            out = []
                            for which, b in ((0, bg), (1, bu)):
                                for kc in range(KC):
                                    off = (which * KC + kc) * 128
                                    out.append(E.matmul(psum[:, b, :], lhsT=ring[:, slot, off:off + 128],
                                                        rhs=hT[:, kc, :], start=(kc == 0), stop=(kc == KC - 1)))
                            return out
                        sc.op("pe", mmgu, r=[("ring", slot)] + hT_keys(hp), w=[("ps", bg), ("ps", bu)])
                        jj = fc % 2
                        sc.op("act", lambda E, bg=bg, jj=jj: E.activation(out=sg[:, jj, :], in_=psum[:, bg, :], func=AF.Silu),
                              r=[("ps", bg)], w=[("sg", jj)])
                        sc.op("dve", lambda E, bu=bu, jj=jj, fc=fc: E.tensor_tensor(out=act[:, fc, :], in0=sg[:, jj, :],
                                                                                 in1=psum[:, bu, :], op=ALU.mult),
                              r=[("sg", jj), ("ps", bu)], w=[("act", fc)])
                    if pre is not None:
                        pre[1][2][1]()
                        pre[1][3][1]()
                    sc.op("dve", lambda E: E.memset(stats[:, 24:44], 0.0),
                          w=[("ssp", s, n) for s in range(4) for n in range(4)])
                    def consume(n, s, b):
                        sc.op("dve", lambda E: E.tensor_copy(out=XB[:, s, n * 512:(n + 1) * 512], in_=psum[:, b, :]),
                              r=[("ps", b)], w=[("XB", s)])
                        sc.op("act", lambda E: E.activation(out=junk[:, 0:512], in_=XB[:, s, n * 512:(n + 1) * 512],
                                                            func=AF.Square,
                                                            accum_out=stats[:, 24 + 4 * s + n:25 + 4 * s + n]),
                              r=[("XB", s)], w=["junk", ("ssp", s, n)])
                    chunks = [wd_b[f][c] for c in range(16)]
                    linear_N(lambda kc, s: act[:, kc, s * 128:(s + 1) * 128], FC, chunks, ("dn", f), 4, 11, consume,
                             lambda s: [("act", fc) for fc in range(FC)])
                    pend = epilogue_parts(t0, XB, XS, gout, stats, junk, src_dram, 0.5, "pool")
                for p_ in pend:
                    p_()
                sc.barrier()

        def ple_stage(li, src_dram):
            cast(("pg", li), pg_b[li], pg_in[li])
            cast(("pp", li), pp_b[li], pp_in[li])
            with ExitStack() as st:
                def sb(name, shape, dt):
                    uid[0] += 1
                    return st.enter_context(nc.sbuf_tensor(f"{name}_{uid[0]}", shape, dt))
                XB = sb("XB", [128, 4, D], F32)
                XS = sb("XS", [128, 2, D], F32)
                xn = sb("xn", [128, 2, D], BF16)
                hT2 = sb("hT", [128, 2, KC, TT], BF16)
                XL = sb("XL", [128, 2, D], F32)
                gin = sb("gin", [128, D], F32)
                gout = sb("gout", [128, D], F32)
                sgm = sb("sgm", [128, 2, TT], F32)
                junk = sb("junk", [128, D], BF16)
                stats = sb("stats", [128, 64], F32)
                wpp = sb("wpp", [128, 2, D], BF16)
                pt = sb("pt", [128, 2, 256], F32)
                ptb = sb("ptb", [128, 2, 256], BF16)
                pT = sb("pT", [128, 2, TT], BF16)
                sc.op("sp", lambda E: [E.dma_start(out=wpp[:], in_=pp_b[li].rearrange("p (k c) -> p k c", k=2))],
                      r=[("wb", ("pp", li))], w=["wpp"], dma=("misc",))
                sc.op("sp", lambda E: [E.dma_start(out=gin[:], in_=ng_in[li, 6:7, :].partition_broadcast(128))],
                      w=["gin"], dma=("misc",))
                sc.op("sp", lambda E: [E.dma_start(out=gout[:], in_=ng_in[li, 7:8, :].partition_broadcast(128))],
                      w=["gout"], dma=("misc",))
                load_norm_T(st, src_dram, 0, None, XL, xn, hT2[:, 0], gin, stats, junk, nxb=2, hp=0, ioq="pool")
                for tt in range(NTT):
                    t0 = tt * TT
                    hp = tt % 2
                    hT = hT2[:, hp]
                    sc.op("dve", lambda E: E.memset(stats[:, 24:44], 0.0),
                          w=[("ssp", s, n) for s in range(4) for n in range(4)])
                    for s in range(4):
                        r0 = t0 + s * 128
                        j = s % 2
                        sc.op("sp", lambda E, j=j, r0=r0: [E.dma_start(out=pt[:, j, :], in_=p_in[li, r0:r0 + 128, :])],
                              w=[("pt", j)], dma=("pt", j))
                        sc.op("pool", lambda E, j=j: E.tensor_copy(out=ptb[:, j, :], in_=pt[:, j, :]),
                              r=[("pt", j)], w=[("ptb", j)])
                        b = next_bank()
                        def tr(E, b=b, j=j):
                            return [E.transpose(out=psb[:, b, q * 128:(q + 1) * 128], in_=ptb[:, j, q * 128:(q + 1) * 128],
                                                identity=ident[:]) for q in range(2)]
                        sc.op("pe", tr, r=[("ptb", j), "ident"], w=[("ps", b)])
                        sc.op("act", lambda E, b=b, s=s: E.copy(out=pT[:, :, s * 128:(s + 1) * 128],
                                                                in_=psb[:, b, 0:256].rearrange("p (q t) -> p q t", q=2)),
                              r=[("ps", b)], w=[("pT", s)])
                    def consume(n, s, b):
                        jj = (n * 4 + s) % 2
                        sc.op("act", lambda E: E.activation(out=sgm[:, jj, :], in_=psum[:, b, :], func=AF.Sigmoid),
                              r=[("ps", b)], w=[("sgm", jj)])
                        b2 = next_bank()
                        def mm2(E):
                            return [E.matmul(psum[:, b2, :], lhsT=pT[:, q, s * 128:(s + 1) * 128],
                                             rhs=wpp[:, q, n * 512:(n + 1) * 512], start=(q == 0), stop=(q == 1))
                                    for q in range(2)]
                        sc.op("pe", mm2, r=[("pT", s), "wpp"], w=[("ps", b2)])
                        sc.op("dve", lambda E: E.tensor_tensor(out=XB[:, s, n * 512:(n + 1) * 512], in0=sgm[:, jj, :],
                                                               in1=psum[:, b2, :], op=ALU.mult),
                              r=[("sgm", jj), ("ps", b2)], w=[("XB", s)])
                        sc.op("act", lambda E: E.activation(out=junk[:, 0:512], in_=XB[:, s, n * 512:(n + 1) * 512],
                                                            func=AF.Square,
                                                            accum_out=stats[:, 24 + 4 * s + n:25 + 4 * s + n]),
                              r=[("XB", s)], w=["junk", ("ssp", s, n)])
                    chunks = [pg_b[li][c] for c in range(8)]
                    linear_N(lambda kc, s: hT[:, kc, s * 128:(s + 1) * 128], KC, chunks, ("pg", li), 4, 8, consume,
                             lambda s: hT_keys(hp))
                    if tt + 1 < NTT:
                        load_norm_T(st, src_dram, t0 + TT, None, XL, xn, hT2[:, 1 - hp], gin, stats, junk, nxb=2, hp=1 - hp, ioq="pool")
                    epilogue(t0, XB, XS, gout, stats, junk, src_dram, 1.0, "pool")
                sc.barrier()

        GAM = [1.0 - 2.0 ** (-5.0 - h) for h in range(4)]

        def gelu_ops(b, dst, dst_keys, gx, gt, gs, kk):
            sc.op("act", lambda E: E.copy(out=gx[:, kk, :], in_=psum[:, b, :]), r=[("ps", b)], w=[("gx", kk)])
            sc.op("pool", lambda E: E.tensor_tensor(out=gt[:, kk, :], in0=gx[:, kk, :], in1=gx[:, kk, :], op=ALU.mult),
                  r=[("gx", kk)], w=[("gt", kk)])
            sc.op("pool", lambda E: E.tensor_scalar(out=gt[:, kk, :], in0=gt[:, kk, :], scalar1=0.044715, scalar2=1.0,
                                                    op0=ALU.mult, op1=ALU.add), w=[("gt", kk)])
            sc.op("pool", lambda E: E.tensor_tensor(out=gt[:, kk, :], in0=gt[:, kk, :], in1=gx[:, kk, :], op=ALU.mult),
                  r=[("gx", kk)], w=[("gt", kk)])
            sc.op("act", lambda E: E.activation(out=gs[:, kk, :], in_=gt[:, kk, :], func=AF.Sigmoid,
                                                scale=1.5957691216057308), r=[("gt", kk)], w=[("gs", kk)])
            sc.op("dve", lambda E: E.tensor_tensor(out=dst, in0=gx[:, kk, :], in1=gs[:, kk, :], op=ALU.mult),
                  r=[("gx", kk), ("gs", kk)], w=dst_keys)

        def rstd_ops(src_ap, width, col, stats, junk, key):
            sc.op("dve", lambda E: E.memset(stats[:, col:col + 1], 0.0), w=[(key, "a")])
            sc.op("act", lambda E: E.activation(out=junk[:, 0:width], in_=src_ap, func=AF.Square,
                                                accum_out=stats[:, col:col + 1]), r=[(key, "src")], w=["junk", (key, "a")])
            sc.op("act", lambda E: E.activation(out=stats[:, col + 1:col + 2], in_=stats[:, col:col + 1], func=AF.Sqrt,
                                                bias=epsc[:, 0:1], scale=1.0 / width), r=[(key, "a"), "epsc"], w=[(key, "b")])
            sc.op("dve", lambda E: E.reciprocal(out=stats[:, col + 2:col + 3], in_=stats[:, col + 1:col + 2]),
                  r=[(key, "b")], w=[(key, "r")])

        def mixer_out_phase(li, src_dram, seq):
            with ExitStack() as st:
                def sb(name, shape, dt):
                    uid[0] += 1
                    return st.enter_context(nc.sbuf_tensor(f"{name}_{uid[0]}", shape, dt))
                XB = sb("XB", [128, 4, D], F32)
                XS = sb("XS", [128, 2, D], F32)
                ybt = sb("ybt", [128, 4, D], BF16)
                yT2 = sb("yT", [128, 2, KC, TT], BF16)
                gout = sb("gout", [128, D], F32)
                junk = sb("junk", [128, D], BF16)
                stats = sb("stats", [128, 64], F32)
                sc.op("sp", lambda E: [E.dma_start(out=gout[:], in_=ng_in[li, 3:4, :].partition_broadcast(128))],
                      w=["gout"], dma=("misc",))
                def y_load_T(t0_, hp_):
                    for s in range(4):
                        r0 = t0_ + s * 128
                        sc.op("sp", lambda E, s=s, r0=r0: [E.dma_start(out=ybt[:, s, :], in_=y_d[r0:r0 + 128, :])],
                              r=[("yd", r0 // 128)], w=[("ybt", s)], dma=("ybt", s))
                        transpose_T(lambda kc, s=s: ybt[:, s, kc * 128:(kc + 1) * 128], [("ybt", s)], yT2[:, hp_], s, hp_)
                y_load_T(seq * S, 0)
                for tq in range(S // TT):
                    t0 = seq * S + tq * TT
                    hp = tq % 2
                    yT = yT2[:, hp]
                    sc.op("dve", lambda E: E.memset(stats[:, 0:44], 0.0),
                          w=[("ssp", s, n) for s in range(4) for n in range(4)])
                    def consume(n, s, b):
                        sc.op("dve", lambda E: E.tensor_copy(out=XB[:, s, n * 512:(n + 1) * 512], in_=psum[:, b, :]),
                              r=[("ps", b)], w=[("XB", s)])
                        sc.op("act", lambda E: E.activation(out=junk[:, 0:512], in_=XB[:, s, n * 512:(n + 1) * 512],
                                                            func=AF.Square,
                                                            accum_out=stats[:, 24 + 4 * s + n:25 + 4 * s + n]),
                              r=[("XB", s)], w=["junk", ("ssp", s, n)])
                    chunks = [wo_b[li][c] for c in range(8)]
                    linear_N(lambda kc, s: yT[:, kc, s * 128:(s + 1) * 128], KC, chunks, ("wo", li), 4, 8, consume,
                             lambda s: hT_keys(hp))
                    if tq + 1 < S // TT:
                        y_load_T(t0 + TT, 1 - hp)
                    epilogue(t0, XB, XS, gout, stats, junk, src_dram, 1.0, "pool")
            sc.barrier()

        def even_mixer_stage(li, src_dram):
            cast(("wi1", li), wi1_b[li], wi1_in[li])
            cast(("wi2", li), wi2_b[li], wi2_in[li])
            cast(("wo", li), wo_b[li], wo_in[li])
            for seq in range(NB):
                with ExitStack() as st:
                    def sb(name, shape, dt):
                        uid[0] += 1
                        return st.enter_context(nc.sbuf_tensor(f"{name}_{uid[0]}", shape, dt))
                    XB = sb("XB", [128, 2, D], F32)
                    xn = sb("xn", [128, 2, D], BF16)
                    hT = sb("hT", [128, KC, TT], BF16)
                    gin = sb("gin", [128, D], F32)
                    junk = sb("junk", [128, D], BF16)
                    stats = sb("stats", [128, 64], F32)
                    qkf = sb("qkf", [128, 2, 2, TT], F32)
                    rq = sb("rq", [128, 16, TT], BF16)
                    cs = sb("cs", [128, 2, S], F32)
                    rt = sb("rt", [128, 4, TT], F32)
                    vgt = sb("vgt", [128, 4, 4096], BF16)
                    gx = sb("gx", [128, 2, TT], F32)
                    gt = sb("gt", [128, 2, TT], F32)
                    gs = sb("gs", [128, 2, TT], F32)
                    gf = sb("gf", [128, 2, TT], F32)
                    sgain = sb("sgain", [128, 1024], F32)
                    sc.op("sp", lambda E: [E.dma_start(out=gin[:], in_=ng_in[li, 2:3, :].partition_broadcast(128))],
                          w=["gin"], dma=("misc",))
                    sc.op("sp", lambda E: [E.dma_start(out=cs[:], in_=c_rot.rearrange("a p t -> p a t"))],
                          w=["cs"], dma=("misc",))
                    sc.op("sp", lambda E: [E.dma_start(out=sgain[:], in_=sgug_in[li].partition_broadcast(128))],
                          w=["sgain"], dma=("misc",))
                    for tq in range(S // TT):
                        t0 = seq * S + tq * TT
                        ts0 = tq * TT
                        load_norm_T(st, src_dram, t0, None, XB, xn, hT, gin, stats, junk, nxb=2)
                        for c in range(8):
                            slot = ring_load(wi1_b[li][c], 4096, ("wi1", li))
                            pp_ = c % 2
                            for fl in range(2):
                                b = next_bank(0, 4)
                                def mm(E, slot=slot, b=b, fl=fl):
                                    return [E.matmul(psum[:, b, :], lhsT=ring[:, slot, (fl * KC + kc) * 128:(fl * KC + kc + 1) * 128],
                                                     rhs=hT[:, kc, :], start=(kc == 0), stop=(kc == KC - 1)) for kc in range(KC)]
                                sc.op("pe", mm, r=[("ring", slot)] + hT_keys(), w=[("ps", b)])
                                if fl == 0:
                                    sc.op("act", lambda E, b=b, pp_=pp_: E.copy(out=qkf[:, pp_, 0, :], in_=psum[:, b, :]),
                                          r=[("ps", b)], w=[("qkf", pp_, 0)])
                                else:
                                    sc.op("dve", lambda E, b=b, pp_=pp_: E.tensor_copy(out=qkf[:, pp_, 1, :], in_=psum[:, b, :]),
                                          r=[("ps", b)], w=[("qkf", pp_, 1)])
                            A = qkf[:, pp_, 0, :]
                            Bq = qkf[:, pp_, 1, :]
                            co = cs[:, 0, ts0:ts0 + TT]
                            si = cs[:, 1, ts0:ts0 + TT]
                            kA = [("qkf", pp_, 0)]
                            kB = [("qkf", pp_, 1)]
                            sc.op("pool", lambda E, A=A, co=co: E.tensor_tensor(out=rt[:, 0, :], in0=A, in1=co, op=ALU.mult),
                                  r=kA + ["cs"], w=[("rt", 0)])
                            sc.op("pool", lambda E, Bq=Bq, si=si: E.tensor_tensor(out=rt[:, 1, :], in0=Bq, in1=si, op=ALU.mult),
                                  r=kB + ["cs"], w=[("rt", 1)])
                            sc.op("dve", lambda E, c=c: E.tensor_tensor(out=rq[:, 2 * c, :], in0=rt[:, 0, :], in1=rt[:, 1, :],
                                                                       op=ALU.subtract),
                                  r=[("rt", 0), ("rt", 1)], w=[("rq", 2 * c)])
                            sc.op("pool", lambda E, Bq=Bq, co=co: E.tensor_tensor(out=rt[:, 2, :], in0=Bq, in1=co, op=ALU.mult),
                                  r=kB + ["cs"], w=[("rt", 2)])
                            sc.op("pool", lambda E, A=A, si=si: E.tensor_tensor(out=rt[:, 3, :], in0=A, in1=si, op=ALU.mult),
                                  r=kA + ["cs"], w=[("rt", 3)])
                            sc.op("dve", lambda E, c=c: E.tensor_tensor(out=rq[:, 2 * c + 1, :], in0=rt[:, 2, :], in1=rt[:, 3, :],
                                                                       op=ALU.add),
                                  r=[("rt", 2), ("rt", 3)], w=[("rq", 2 * c + 1)])
                        kcount = [0]
                        def consume(n, s, b):
                            dst = vgt[:, s, n * 512:(n + 1) * 512]
                            dk = [("vgt", s)]
                            if n < 2:
                                sc.op("act", lambda E: E.copy(out=dst, in_=psum[:, b, :]), r=[("ps", b)], w=dk)
                            elif n < 4:
                                sc.op("act", lambda E: E.activation(out=dst, in_=psum[:, b, :], func=AF.Silu),
                                      r=[("ps", b)], w=dk)
                            elif n < 6:
                                kk = kcount[0] % 2
                                kcount[0] += 1
                                gelu_ops(b, dst, dk, gx, gt, gs, kk)
                            else:
                                kk = kcount[0] % 2
                                kcount[0] += 1
                                gelu_ops(b, gf[:, kk, :], [("gf", kk)], gx, gt, gs, kk)
                                for g2 in range(2):
                                    grp = (n - 6) * 2 + g2
                                    key = ("vsn", kk, g2)
                                    col = 52 + 3 * (2 * kk + g2)
                                    sc.op("dve", lambda E: E.memset(stats[:, col:col + 1], 0.0), w=[(key, "a")])
                                    sc.op("act", lambda E: E.activation(out=junk[:, 0:256], in_=gf[:, kk, g2 * 256:(g2 + 1) * 256],
                                                                        func=AF.Square, accum_out=stats[:, col:col + 1]),
                                          r=[("gf", kk)], w=["junk", (key, "a")])
                                    sc.op("act", lambda E: E.activation(out=stats[:, col + 1:col + 2], in_=stats[:, col:col + 1],
                                                                        func=AF.Sqrt, bias=epsc[:, 0:1], scale=1.0 / 256),
                                          r=[(key, "a"), "epsc"], w=[(key, "b")])
                                    sc.op("dve", lambda E: E.reciprocal(out=stats[:, col + 2:col + 3], in_=stats[:, col + 1:col + 2]),
                                          r=[(key, "b")], w=[(key, "r")])
                                    sc.op("dve", lambda E: E.scalar_tensor_tensor(
                                        out=vgt[:, s, n * 512 + g2 * 256:n * 512 + (g2 + 1) * 256],
                                        in0=gf[:, kk, g2 * 256:(g2 + 1) * 256], scalar=stats[:, col + 2:col + 3],
                                        in1=sgain[:, grp * 256:(grp + 1) * 256], op0=ALU.mult, op1=ALU.mult),
                                        r=[("gf", kk), (key, "r"), "sgain"], w=dk)
                        chunks = [wi2_b[li][c] for c in range(16)]
                        linear_N(lambda kc, s: hT[:, kc, s * 128:(s + 1) * 128], KC, chunks, ("wi2", li), 8, 8, consume,
                                 lambda s: hT_keys())
                        sc.op("pool", lambda E, t0=t0: [E.dma_start(out=qk_d[:, :, t0:t0 + TT].rearrange("c p t -> p c t"),
                                                                  in_=rq[:])],
                              r=[("rq", c) for c in range(16)], w=[("qkd", t0 // TT)], dma=("rqst",))
                        for s in range(4):
                            r0 = t0 + s * 128
                            sc.op("pool", lambda E, s=s, r0=r0: [E.dma_start(out=vg_d[r0:r0 + 128, :], in_=vgt[:, s, :])],
                                  r=[("vgt", s)], w=[("vgd", r0 // 128)], dma=("vgst", s))
                sc.barrier()
                with ExitStack() as st:
                    def sb(name, shape, dt):
                        uid[0] += 1
                        return st.enter_context(nc.sbuf_tensor(f"{name}_{uid[0]}", shape, dt))
                    kTh = sb("kTh", [128, 2, S], BF16)
                    vh = sb("vh", [128, 16, 256], BF16)
                    g2t = sb("g2t", [128, 1024], F32)
                    qTt = sb("qTt", [128, 2, 2, TT], BF16)
                    PT = sb("PT", [128, 3, TT], BF16)
                    osb = sb("osb", [128, 2, 256], F32)
                    sgt = sb("sgt", [128, 2, 4, 256], BF16)
                    yt = sb("yt", [128, 2, 4, 256], BF16)
                    stats = sb("stats", [128, 64], F32)
                    junk = sb("junk", [128, 512], BF16)
                    wsf = sb("wsf", [128, 4, 128], F32)
                    wsb = sb("wsb", [128, 4, 128], BF16)
                    tri = sb("tri", [128, 128], F32)
                    bT = sb("bT", [128, 4], F32)
                    vnt = sb("vnt", [128, 2, 1024], BF16)
                    gut = sb("gut", [128, 2, 1024], BF16)
                    y2 = sb("y2", [128, 2, 1024], BF16)
                    r00 = seq * S
                    pti = [0]
                    for h in range(4):
                        sc.op("sp", lambda E, h=h: [E.dma_start(out=kTh[:], in_=qk_d[8 + 2 * h:10 + 2 * h, :, r00:r00 + S]
                                                                 .rearrange("c p t -> p c t"))],
                              w=["kTh"], dma=("misc",))
                        sc.op("sp", lambda E, h=h: [E.dma_start(out=vh[:], in_=vg_d[r00:r00 + S, h * 256:(h + 1) * 256]
                                                                 .rearrange("(j p) e -> p j e", p=128))],
                              w=["vh"], dma=("misc",))
                        sc.op("sp", lambda E, h=h: [E.dma_start(out=g2t[:], in_=c_g2[h])], w=["g2t"], dma=("misc",))
                        for T in range(4):
                            tp = T % 2
                            t0 = r00 + T * TT
                            sc.op("sp", lambda E, h=h, t0=t0, tp=tp: [E.dma_start(
                                out=qTt[:, tp], in_=qk_d[2 * h:2 * h + 2, :, t0:t0 + TT].rearrange("c p t -> p c t"))],
                                w=[("qTt", tp)], dma=("qTt", tp))
                            sc.op("sp", lambda E, h=h, t0=t0, tp=tp: [E.dma_start(
                                out=sgt[:, tp], in_=vg_d[t0:t0 + TT, 1024 + h * 256:1024 + (h + 1) * 256]
                                .rearrange("(i p) e -> p i e", p=128))], w=[("sgt", tp)], dma=("sgt", tp))
                            nj = 4 * T + 4
                            pend_pv = None
                            for j in range(nj):
                                b = next_bank(0, 4)
                                def smm(E, b=b, j=j, tp=tp):
                                    return [E.matmul(psum[:, b, :], lhsT=kTh[:, c, j * 128:(j + 1) * 128], rhs=qTt[:, tp, c, :],
                                                     start=(c == 0), stop=(c == 1)) for c in range(2)]
                                sc.op("pe", smm, r=["kTh", ("qTt", tp)], w=[("ps", b)])
                                if pend_pv is not None:
                                    pend_pv()
                                    pend_pv = None
                                dl = 4 * T - j
                                if dl >= 1:
                                    off = 512
                                    cc = GAM[h] ** (128.0 * (dl - 1))
                                    jp = -1
                                else:
                                    jp = -dl
                                    off = 384 - 128 * jp
                                    cc = 1.0
                                pk = pti[0] % 3
                                pti[0] += 1
                                sc.op("dve", lambda E, b=b, off=off, cc=cc, pk=pk: E.scalar_tensor_tensor(
                                    out=PT[:, pk, :], in0=psum[:, b, :], scalar=float(cc), in1=g2t[:, off:off + 512],
                                    op0=ALU.mult, op1=ALU.mult), r=[("ps", b), "g2t"], w=[("PT", pk)])
                                subs = [i for i in range(4) if i >= jp]
                                def pv(E, pk=pk, j=j, subs=subs, T=T):
                                    return [E.matmul(psum[:, 4 + i, 0:256], lhsT=PT[:, pk, i * 128:(i + 1) * 128], rhs=vh[:, j, :],
                                                     start=(j == 0), stop=(j == 4 * T + i)) for i in subs]
                                def emit_pv(pv=pv, pk=pk, subs=subs):
                                    sc.op("pe", pv, r=[("PT", pk), "vh"], w=[("ps", 4 + i) for i in subs])
                                pend_pv = emit_pv
                            pend_pv()
                            for i in range(4):
                                ok = i % 2
                                sc.op("dve", lambda E, i=i, ok=ok: E.tensor_copy(out=osb[:, ok, :], in_=psum[:, 4 + i, 0:256]),
                                      r=[("ps", 4 + i)], w=[("osb", ok)])
                                col = 3 * ok
                                key = ("ron", ok)
                                sc.op("dve", lambda E, col=col: E.memset(stats[:, col:col + 1], 0.0), w=[(key, "a")])
                                sc.op("act", lambda E, ok=ok, col=col: E.activation(out=junk[:, 0:256], in_=osb[:, ok, :],
                                                                                  func=AF.Square, accum_out=stats[:, col:col + 1]),
                                      r=[("osb", ok)], w=["junk", (key, "a")])
                                sc.op("act", lambda E, col=col: E.activation(out=stats[:, col + 1:col + 2], in_=stats[:, col:col + 1],
                                                                           func=AF.Sqrt, bias=epsc[:, 0:1], scale=1.0 / 256),
                                      r=[(key, "a"), "epsc"], w=[(key, "b")])
                                sc.op("dve", lambda E, col=col: E.reciprocal(out=stats[:, col + 2:col + 3], in_=stats[:, col + 1:col + 2]),
                                      r=[(key, "b")], w=[(key, "r")])
                                sc.op("dve", lambda E, i=i, ok=ok, col=col, tp=tp: E.scalar_tensor_tensor(
                                    out=yt[:, tp, i, :], in0=osb[:, ok, :], scalar=stats[:, col + 2:col + 3], in1=sgt[:, tp, i, :],
                                    op0=ALU.mult, op1=ALU.mult), r=[("osb", ok), (key, "r"), ("sgt", tp)], w=[("yt", tp)])
                            sc.op("pool", lambda E, h=h, t0=t0, tp=tp: [E.dma_start(
                                out=y_d[t0:t0 + TT, h * 256:(h + 1) * 256].rearrange("(i p) e -> p i e", p=128), in_=yt[:, tp])],
                                r=[("yt", tp)], w=[("yd", t0 // 128 + i) for i in range(4)], dma=("ytst", tp))
                    sc.op("sp", lambda E: [E.dma_start(out=wsf[:], in_=sguw_in[li])], w=["wsf"], dma=("misc",))
                    sc.op("sp", lambda E: [E.dma_start(out=tri[:], in_=c_tri)], w=["tri"], dma=("misc",))
                    sc.op("sp", lambda E: [E.dma_start(out=bT[:], in_=sgub_in[li])], w=["bT"], dma=("misc",))
                    for g in range(4):
                        sc.op("dve", lambda E, g=g: E.tensor_tensor(out=wsb[:, g, :], in0=wsf[:, g, :], in1=tri[:], op=ALU.mult),
                              r=["wsf", "tri"], w=["wsb"])
                    for c in range(16):
                        r0 = r00 + c * 128
                        cp = c % 2
                        sc.op("sp", lambda E, r0=r0, cp=cp: [E.dma_start(out=vnt[:, cp, :], in_=vg_d[r0:r0 + 128, 3072:4096])],
                              w=[("vnt", cp)], dma=("vnt", cp))
                        sc.op("sp", lambda E, r0=r0, cp=cp: [E.dma_start(out=gut[:, cp, :], in_=vg_d[r0:r0 + 128, 2048:3072])],
                              w=[("gut", cp)], dma=("gut", cp))
                        bA = next_bank(0, 4)
                        bB = next_bank(0, 4)
                        def sm(E, cp=cp, bA=bA, bB=bB):
                            out = []
                            for g in range(4):
                                bb = bA if g < 2 else bB
                                out.append(E.matmul(psum[:, bb, (g % 2) * 256:(g % 2 + 1) * 256], lhsT=wsb[:, g, :],
                                                    rhs=vnt[:, cp, g * 256:(g + 1) * 256], start=True, stop=True))
                            return out
                        sc.op("pe", sm, r=["wsb", ("vnt", cp)], w=[("ps", bA), ("ps", bB)])
                        for g in range(4):
                            bb = bA if g < 2 else bB
                            sc.op("dve", lambda E, g=g, bb=bb, cp=cp: E.scalar_tensor_tensor(
                                out=y2[:, cp, g * 256:(g + 1) * 256], in0=psum[:, bb, (g % 2) * 256:(g % 2 + 1) * 256],
                                scalar=bT[:, g:g + 1], in1=gut[:, cp, g * 256:(g + 1) * 256], op0=ALU.add, op1=ALU.mult),
                                r=[("ps", bb), "bT", ("gut", cp)], w=[("y2", cp)])
                        sc.op("pool", lambda E, r0=r0, cp=cp: [E.dma_start(out=y_d[r0:r0 + 128, 1024:2048], in_=y2[:, cp, :])],
                              r=[("y2", cp)], w=[("yd", r0 // 128)], dma=("y2st", cp))
                sc.barrier()
                mixer_out_phase(li, src_dram, seq)

        def odd_mixer_stage(li, src_dram):
            cast(("wi1", li), wi1_b[li], wi1_in[li])
            cast(("wi2", li), wi2_b[li], wi2_in[li])
            cast(("wo", li), wo_b[li], wo_in[li])
            SCL = 128.0 ** -0.5
            for seq in range(NB):
                r00 = seq * S
                with ExitStack() as st:
                    def sb(name, shape, dt):
                        uid[0] += 1
                        return st.enter_context(nc.sbuf_tensor(f"{name}_{uid[0]}", shape, dt))
                    XB = sb("XB", [128, 2, D], F32)
                    xn = sb("xn", [128, 2, D], BF16)
                    hT = sb("hT", [128, KC, TT], BF16)
                    gin = sb("gin", [128, D], F32)
                    junk = sb("junk", [128, D], BF16)
                    stats = sb("stats", [128, 64], F32)
                    qf = sb("qf", [128, 2, TT], F32)
                    qb = sb("qb", [128, 2, TT], BF16)
                    rq = sb("rq", [128, 16, TT], BF16)
                    cs = sb("cs", [128, 2, S], F32)
                    rt = sb("rt", [128, 2, 2, TT], F32)
                    rf = sb("rf", [128, 2, TT], F32)
                    pvt = sb("pvt", [128, 4, D], BF16)
                    permf = sb("permf", [128, 128], F32)
                    permb = sb("permb", [128, 128], BF16)
                    kms = sb("kms", [128, 8, 8], F32)
                    kmb = sb("kmb", [128, 8, 8], BF16)
                    sc.op("sp", lambda E: [E.dma_start(out=gin[:], in_=ng_in[li, 2:3, :].partition_broadcast(128))],
                          w=["gin"], dma=("misc",))
                    sc.op("sp", lambda E: [E.dma_start(out=cs[:], in_=c_rot2.rearrange("a p t -> p a t"))],
                          w=["cs"], dma=("misc",))
                    sc.op("sp", lambda E: [E.dma_start(out=permf[:], in_=c_perm)], w=["permf"], dma=("misc",))
                    sc.op("dve", lambda E: E.tensor_copy(out=permb[:], in_=permf[:]), r=["permf"], w=["permb"])
                    for tq in range(S // TT):
                        t0 = r00 + tq * TT
                        ts0 = tq * TT
                        load_norm_T(st, src_dram, t0, None, XB, xn, hT, gin, stats, junk, nxb=2)
                        for c in range(8):
                            slot = ring_load(wi1_b[li][c], 4096, ("wi1", li))
                            for fl in range(2):
                                fc = 2 * c + fl
                                pq = fc % 2
                                b = next_bank(0, 4)
                                def mm(E, slot=slot, b=b, fl=fl):
                                    return [E.matmul(psum[:, b, :], lhsT=ring[:, slot, (fl * KC + kc) * 128:(fl * KC + kc + 1) * 128],
                                                     rhs=hT[:, kc, :], start=(kc == 0), stop=(kc == KC - 1)) for kc in range(KC)]
                                sc.op("pe", mm, r=[("ring", slot)] + hT_keys(), w=[("ps", b)])
                                sc.op("act", lambda E: E.copy(out=qf[:, pq, :], in_=psum[:, b, :]), r=[("ps", b)], w=[("qf", pq)])
                                sc.op("dve", lambda E: E.tensor_copy(out=qb[:, pq, :], in_=qf[:, pq, :]), r=[("qf", pq)], w=[("qb", pq)])
                                b2 = next_bank(0, 4)
                                sc.op("pe", lambda E: E.matmul(psum[:, b2, :], lhsT=permb[:], rhs=qb[:, pq, :], start=True, stop=True),
                                      r=["permb", ("qb", pq)], w=[("ps", b2)])
                                sc.op("pool", lambda E: E.tensor_tensor(out=rt[:, pq, 0, :], in0=qf[:, pq, :],
                                                                        in1=cs[:, 0, ts0:ts0 + TT], op=ALU.mult),
                                      r=[("qf", pq), "cs"], w=[("rt", pq, 0)])
                                sc.op("dve", lambda E: E.tensor_tensor(out=rt[:, pq, 1, :], in0=psum[:, b2, :],
                                                                       in1=cs[:, 1, ts0:ts0 + TT], op=ALU.mult),
                                      r=[("ps", b2), "cs"], w=[("rt", pq, 1)])
                                if fc < 8:
                                    sc.op("dve", lambda E: E.tensor_tensor(out=rq[:, fc, :], in0=rt[:, pq, 0, :], in1=rt[:, pq, 1, :],
                                                                           op=ALU.add),
                                          r=[("rt", pq, 0), ("rt", pq, 1)], w=[("rq", fc)])
                                else:
                                    sc.op("dve", lambda E: E.tensor_tensor(out=rf[:, pq, :], in0=rt[:, pq, 0, :], in1=rt[:, pq, 1, :],
                                                                           op=ALU.add),
                                          r=[("rt", pq, 0), ("rt", pq, 1)], w=[("rf", pq)])
                                    sc.op("act", lambda E: E.copy(out=rq[:, fc, :], in_=rf[:, pq, :]), r=[("rf", pq)], w=[("rq", fc)])
                                    sc.op("dve", lambda E: E.tensor_reduce(
                                        out=kms[:, fc - 8, 2 * tq:2 * tq + 2], in_=rf[:, pq, :].rearrange("p (b t) -> p b t", b=2),
                                        axis=AX.X, op=ALU.add), r=[("rf", pq)], w=["kms"])
                        def consume(n, s, b):
                            dst = pvt[:, s, n * 512:(n + 1) * 512]
                            if (n + s) % 2 == 0:
                                sc.op("act", lambda E: E.copy(out=dst, in_=psum[:, b, :]), r=[("ps", b)], w=[("pvt", s)])
                            else:
                                sc.op("dve", lambda E: E.tensor_copy(out=dst, in_=psum[:, b, :]), r=[("ps", b)], w=[("pvt", s)])
                        chunks = [wi2_b[li][c] for c in range(8)]
                        linear_N(lambda kc, s: hT[:, kc, s * 128:(s + 1) * 128], KC, chunks, ("wi2", li), 4, 8, consume,
                                 lambda s: hT_keys())
                        sc.op("pool", lambda E, t0=t0: [E.dma_start(out=qk_d[:, :, t0:t0 + TT].rearrange("c p t -> p c t"),
                                                                  in_=rq[:])],
                              r=[("rq", c) for c in range(16)], w=[("qkd", t0 // TT)], dma=("rqst",))
                        for s in range(4):
                            r0 = t0 + s * 128
                            sc.op("pool", lambda E, s=s, r0=r0: [E.dma_start(out=pv_d[r0:r0 + 128, :], in_=pvt[:, s, :])],
                                  r=[("pvt", s)], w=[("pvd", r0 // 128)], dma=("pvst", s))
                    sc.op("dve", lambda E: E.tensor_scalar(out=kmb[:], in0=kms[:], scalar1=1.0 / 256.0, scalar2=None, op0=ALU.mult),
                          r=["kms"], w=["kmb"])
                    sc.op("sp", lambda E: [E.dma_start(out=km_d, in_=kmb[:].rearrange("p h n -> p (h n)"))],
                          r=["kmb"], w=["kmd"], dma=("misc",))
                sc.barrier()
                OST = int(os.environ.get("KDBG_OSTOP", "99"))
                if OST == 1:
                    continue
                with ExitStack() as st:
                    def sb(name, shape, dt):
                        uid[0] += 1
                        return st.enter_context(nc.sbuf_tensor(f"{name}_{uid[0]}", shape, dt))
                    kTh = sb("kTh", [128, S], BF16)
                    vhe = sb("vhe", [128, 16, 132], BF16)
                    qTt = sb("qTt", [128, 2, TT], BF16)
                    PT = sb("PT", [128, 3, TT], BF16)
                    biasT = sb("biasT", [128, 2, TT], BF16)
                    CTf = sb("CTf", [128, 4, TT], F32)
                    CT = sb("CT", [128, 4, TT], BF16)
                    indf = sb("indf", [128, 8, 128], F32)
                    ind = sb("ind", [128, 8, 128], BF16)
                    kmb = sb("kmb", [128, 8, 8], BF16)
                    negm = sb("negm", [128, 8, 8], F32)
                    bcon = sb("bcon", [128, 4, 8], F32)
                    g8 = sb("g8", [128, 2, 8], F32)
                    top8 = sb("top8", [128, 2, 8], F32)
                    bf_ = sb("bf_", [128, 2, 8], F32)
                    bb_ = sb("bb_", [128, 2, 8], BF16)
                    rec = sb("rec", [128, 2, 1], F32)
                    ym = sb("ym", [128, 2, 4, 128], BF16)
                    sc.op("sp", lambda E: [E.dma_start(out=CTf[:], in_=c_ct.rearrange("j p t -> p j t"))], w=["CTf"], dma=("misc",))
                    sc.op("dve", lambda E: E.tensor_copy(out=CT[:], in_=CTf[:]), r=["CTf"], w=["CT"])
                    sc.op("sp", lambda E: [E.dma_start(out=indf[0:8], in_=c_ind.rearrange("p (n t) -> p n t", n=8))],
                          w=["indf"], dma=("misc",))
                    sc.op("dve", lambda E: E.tensor_copy(out=ind[0:8], in_=indf[0:8]), r=["indf"], w=["ind"])
                    sc.op("sp", lambda E: [E.dma_start(out=negm[:], in_=c_negm.partition_broadcast(128)
                                                        .rearrange("p o (a b) -> p (o a) b", a=8))], w=["negm"], dma=("misc",))
                    sc.op("sp", lambda E: [E.dma_start(out=bcon[:], in_=c_bcon.partition_broadcast(128)
                                                        .rearrange("p o (a b) -> p (o a) b", a=4))], w=["bcon"], dma=("misc",))
                    sc.op("sp", lambda E: [E.dma_start(out=kmb[:], in_=km_d.rearrange("p (h n) -> p h n", h=8))],
                          r=["kmd"], w=["kmb"], dma=("misc",))
                    sc.op("dve", lambda E: E.memset(vhe[:], 1.0), w=["vhe"])
                    pti = [0]
                    for h in range(8):
                        sc.op("sp", lambda E, h=h: [E.dma_start(out=kTh[:], in_=qk_d[8 + h, :, r00:r00 + S])], w=["kTh"], dma=("misc",))
                        sc.op("sp", lambda E, h=h: [E.dma_start(out=vhe[:, :, 0:128], in_=pv_d[r00:r00 + S, 1024 + h * 128:1024 + (h + 1) * 128]
                                                                 .rearrange("(j p) e -> p j e", p=128))], w=["vhe"], dma=("misc",))
                        for T in range(4):
                            tp = T % 2
                            t0 = r00 + T * TT
                            sc.op("sp", lambda E, h=h, t0=t0, tp=tp: [E.dma_start(out=qTt[:, tp, :], in_=qk_d[h, :, t0:t0 + TT])],
                                  w=[("qTt", tp)], dma=("qTt", tp))
                            for i in range(4):
                                qbk = (4 * T + i) // 2
                                ip = i % 2
                                if qbk <= 3:
                                    sc.op("act", lambda E, ip=ip, qbk=qbk: E.copy(out=bb_[:, ip, :], in_=bcon[:, qbk, :]),
                                          r=["bcon"], w=[("bb", ip)])
                                else:
                                    b = next_bank(0, 4)
                                    sc.op("pe", lambda E, b=b, i=i, tp=tp, h=h: E.matmul(
                                        psum[:, b, 0:8], lhsT=qTt[:, tp, i * 128:(i + 1) * 128], rhs=kmb[:, h, :], start=True, stop=True),
                                        r=[("qTt", tp), "kmb"], w=[("ps", b)])
                                    sc.op("dve", lambda E, b=b, ip=ip, qbk=qbk: E.tensor_tensor(
                                        out=g8[:, ip, :], in0=psum[:, b, 0:8], in1=negm[:, qbk, :], op=ALU.add),
                                        r=[("ps", b), "negm"], w=[("g8", ip)])
                                    sc.op("dve", lambda E, ip=ip: E.max(out=top8[:, ip, :], in_=g8[:, ip, :]),
                                          r=[("g8", ip)], w=[("top8", ip)])
                                    sc.op("dve", lambda E, ip=ip: E.tensor_scalar(
                                        out=bf_[:, ip, :], in0=g8[:, ip, :], scalar1=top8[:, ip, 2:3], scalar2=NEGB,
                                        op0=ALU.is_lt, op1=ALU.mult), r=[("g8", ip), ("top8", ip)], w=[("bf", ip)])
                                    sc.op("dve", lambda E, ip=ip, qbk=qbk: E.memset(bf_[:, ip, qbk:qbk + 1], 0.0), w=[("bf", ip)])
                                    sc.op("act", lambda E, ip=ip: E.copy(out=bb_[:, ip, :], in_=bf_[:, ip, :]),
                                          r=[("bf", ip)], w=[("bb", ip)])
                                b = next_bank(0, 4)
                                sc.op("pe", lambda E, b=b, ip=ip: E.transpose(out=psb[0:8, b, 0:128], in_=bb_[:, ip, :], identity=ident[:]),
                                      r=[("bb", ip), "ident"], w=[("ps", b)])
                                sc.op("act", lambda E, b=b, i=i, tp=tp: E.copy(out=biasT[0:8, tp, i * 128:(i + 1) * 128],
                                                                             in_=psb[0:8, b, 0:128]),
                                      r=[("ps", b)], w=[("biasT", tp)])
                            nj = 4 * T + 4
                            pend_pv = None
                            for j in range(nj):
                                b = next_bank(0, 4)
                                jj = j - 4 * T
                                def smm(E, b=b, j=j, tp=tp, jj=jj):
                                    out = [E.matmul(psum[:, b, :], lhsT=kTh[:, j * 128:(j + 1) * 128], rhs=qTt[:, tp, :],
                                                    start=True, stop=False),
                                           E.matmul(psum[:, b, :], lhsT=ind[0:8, j // 2, :], rhs=biasT[0:8, tp, :],
                                                    start=False, stop=(jj < 0))]
                                    if jj >= 0:
                                        out.append(E.matmul(psum[:, b, :], lhsT=ident[:], rhs=CT[:, jj, :], start=False, stop=True))
                                    return out
                                sc.op("pe", smm, r=["kTh", ("qTt", tp), "ind", ("biasT", tp), "CT", "ident"], w=[("ps", b)])
                                if pend_pv is not None:
                                    pend_pv()
                                    pend_pv = None
                                pk = pti[0] % 3
                                pti[0] += 1
                                sc.op("act", lambda E, b=b, pk=pk: E.activation(out=PT[:, pk, :], in_=psum[:, b, :], func=AF.Exp, scale=SCL),
                                      r=[("ps", b)], w=[("PT", pk)])
                                subs = [i for i in range(4) if i >= jj]
                                def pv(E, pk=pk, j=j, subs=subs, T=T):
                                    return [E.matmul(psum[:, 4 + i, 0:129], lhsT=PT[:, pk, i * 128:(i + 1) * 128], rhs=vhe[:, j, 0:129],
                                                     start=(j == 0), stop=(j == 4 * T + i)) for i in subs]
                                def emit_pv(pv=pv, pk=pk, subs=subs):
                                    sc.op("pe", pv, r=[("PT", pk), "vhe"], w=[("ps", 4 + i) for i in subs])
                                pend_pv = emit_pv
                            pend_pv()
                            for i in range(4):
                                ok = i % 2
                                sc.op("dve", lambda E, i=i, ok=ok: E.reciprocal(out=rec[:, ok, :], in_=psum[:, 4 + i, 128:129]),
                                      r=[("ps", 4 + i)], w=[("rec", ok)])
                                sc.op("dve", lambda E, i=i, ok=ok, tp=tp: E.tensor_scalar(
                                    out=ym[:, tp, i, :], in0=psum[:, 4 + i, 0:128], scalar1=rec[:, ok, :], scalar2=None, op0=ALU.mult),
                                    r=[("ps", 4 + i), ("rec", ok)], w=[("ym", tp)])
                            sc.op("pool", lambda E, h=h, t0=t0, tp=tp: [E.dma_start(
                                out=y_d[t0:t0 + TT, 1024 + h * 128:1024 + (h + 1) * 128].rearrange("(i p) e -> p i e", p=128),
                                in_=ym[:, tp])], r=[("ym", tp)], w=[("yd", t0 // 128 + i) for i in range(4)], dma=("ymst", tp))
                sc.barrier()
                if OST == 2:
                    continue
                with ExitStack() as st:
                    def sb(name, shape, dt):
                        uid[0] += 1
                        return st.enter_context(nc.sbuf_tensor(f"{name}_{uid[0]}", shape, dt))
                    apf = sb("apf", [128, 12, 128], F32)
                    apb = sb("apb", [128, 12, 128], BF16)
                    wpf = sb("wpf", [128, 8, 256], F32)
                    wpb = sb("wpb", [128, 8, 256], BF16)
                    psc = sb("psc", [128, 1024], F32)
                    pzt = sb("pzt", [128, 3, 1024], BF16)
                    yTb = sb("yTb", [128, 2, 8, 128], BF16)
                    y1 = sb("y1", [128, 2, 1024], BF16)
                    sc.op("sp", lambda E: [E.dma_start(out=apf[:], in_=c_pool.rearrange("a p t -> p a t"))], w=["apf"], dma=("misc",))
                    sc.op("dve", lambda E: E.tensor_copy(out=apb[:], in_=apf[:]), r=["apf"], w=["apb"])
                    sc.op("sp", lambda E: [E.dma_start(out=wpf[:], in_=poolw_in[li])], w=["wpf"], dma=("misc",))
                    sc.op("dve", lambda E: E.tensor_copy(out=wpb[:], in_=wpf[:]), r=["wpf"], w=["wpb"])
                    sc.op("sp", lambda E: [E.dma_start(out=psc[:], in_=pools_in[li].partition_broadcast(128))], w=["psc"], dma=("misc",))
                    for c in range(16):
                        r0 = r00 + c * 128
                        cp = c % 3
                        pp_ = (c - 1) % 3
                        c2 = c % 2
                        sc.op("sp", lambda E, r0=r0, cp=cp: [E.dma_start(out=pzt[:, cp, :], in_=pv_d[r0:r0 + 128, 0:1024])],
                              w=[("pzt", cp)], dma=("pzt", cp))
                        bA = next_bank(0, 8)
                        bB = next_bank(0, 8)
                        def pm(E, c=c, cp=cp, pp_=pp_, bA=bA, bB=bB):
                            out = []
                            for gi in range(4):
                                for cc in range(2):
                                    idx = gi * 2 + cc
                                    bb = bA if idx < 4 else bB
                                    dst = psum[:, bb, (idx % 4) * 128:(idx % 4 + 1) * 128]
                                    lcur = pzt[:, cp, gi * 256 + cc * 128:gi * 256 + (cc + 1) * 128]
                                    if c == 0:
                                        out.append(E.matmul(dst, lhsT=lcur, rhs=apb[:, 4 + gi, :], start=True, stop=True))
                                    else:
                                        lprev = pzt[:, pp_, gi * 256 + cc * 128:gi * 256 + (cc + 1) * 128]
                                        out.append(E.matmul(dst, lhsT=lcur, rhs=apb[:, gi, :], start=True, stop=False))
                                        out.append(E.matmul(dst, lhsT=lprev, rhs=apb[:, 8 + gi, :], start=False, stop=True))
                            return out
                        sc.op("pe", pm, r=[("pzt", cp), ("pzt", pp_), "apb"], w=[("ps", bA), ("ps", bB)])
                        sc.op("act", lambda E, bA=bA, c2=c2: E.copy(out=yTb[:, c2, 0:4, :], in_=psum[:, bA, :].rearrange("p (a t) -> p a t", a=4)),
                              r=[("ps", bA)], w=[("yTb", c2, 0)])
                        sc.op("dve", lambda E, bB=bB, c2=c2: E.tensor_copy(out=yTb[:, c2, 4:8, :], in_=psum[:, bB, :].rearrange("p (a t) -> p a t", a=4)),
                              r=[("ps", bB)], w=[("yTb", c2, 1)])
                        bC = next_bank(0, 8)
                        bD = next_bank(0, 8)
                        def pl(E, c2=c2, bC=bC, bD=bD):
                            out = []
                            for gi in range(4):
                                bb = bC if gi < 2 else bD
                                for cc in range(2):
                                    out.append(E.matmul(psum[:, bb, (gi % 2) * 256:(gi % 2 + 1) * 256], lhsT=yTb[:, c2, gi * 2 + cc, :],
                                                        rhs=wpb[:, gi * 2 + cc, :], start=(cc == 0), stop=(cc == 1)))
                            return out
                        sc.op("pe", pl, r=[("yTb", c2, 0), ("yTb", c2, 1), "wpb"], w=[("ps", bC), ("ps", bD)])
                        for half, bb in ((0, bC), (1, bD)):
                            sc.op("dve", lambda E, half=half, bb=bb, c2=c2: E.tensor_tensor(
                                out=y1[:, c2, half * 512:(half + 1) * 512], in0=psum[:, bb, :], in1=psc[:, half * 512:(half + 1) * 512],
                                op=ALU.mult), r=[("ps", bb), "psc"], w=[("y1", c2)])
                        sc.op("pool", lambda E, r0=r0, c2=c2: [E.dma_start(out=y_d[r0:r0 + 128, 0:1024], in_=y1[:, c2, :])],
                              r=[("y1", c2)], w=[("yd", r0 // 128)], dma=("y1st", c2))
                sc.barrier()
                mixer_out_phase(li, src_dram, seq)

        for li in layers:
            cast(("gu", li * 2), gu_b[li * 2], gu_in[li * 2])
            cast(("dn", li * 2), wd_b[li * 2], wd_in[li * 2])
            cast(("wi1", li), wi1_b[li], wi1_in[li])
            cast(("wi2", li), wi2_b[li], wi2_in[li])
            cast(("wo", li), wo_b[li], wo_in[li])
            cast(("gu", li * 2 + 1), gu_b[li * 2 + 1], gu_in[li * 2 + 1])
            cast(("dn", li * 2 + 1), wd_b[li * 2 + 1], wd_in[li * 2 + 1])
            cast(("pg", li), pg_b[li], pg_in[li])
            cast(("pp", li), pp_b[li], pp_in[li])
        first = True
        for idx, li in enumerate(layers):
            nst = n_stage if idx == len(layers) - 1 else 4
            src = x_in if first else y_out
            ffn_stage(li, 0, src)
            first = False
            if nst >= 2:
                if li % 2 == 0:
                    even_mixer_stage(li, y_out)
                else:
                    odd_mixer_stage(li, y_out)
            if nst >= 3:
                ffn_stage(li, 1, y_out)
            if nst >= 4:
                ple_stage(li, y_out)
        sc.barrier(engines=("sp",))
        print("ops", sc.nops, "sems", len(sc.sem))
    return nc


def _p1_chunks(W):
    K, F = W.shape
    a = W.reshape(K // 128, 128, F // 256, 2, 128)
    a = a.transpose(2, 1, 3, 0, 4)
    return np.ascontiguousarray(a).reshape(F // 256, 128, -1)


def _p2_chunks(W, kper):
    K, N = W.shape
    ng = K // 128 // kper
    a = W.reshape(ng, kper, 128, N // 512, 512)
    a = a.transpose(3, 0, 2, 1, 4)
    return np.ascontiguousarray(a).reshape(N // 512 * ng, 128, kper * 512)


def _gu_chunks(Wg, Wu):
    K, F = Wg.shape
    g = Wg.reshape(K // 128, 128, F // 128, 128).transpose(2, 1, 0, 3)
    u = Wu.reshape(K // 128, 128, F // 128, 128).transpose(2, 1, 0, 3)
    a = np.stack([g, u], axis=2)
    return np.ascontiguousarray(a).reshape(F // 128, 128, -1)


def prep_weights(inp, layers):
    out = {}
    for li in layers:
        for j in range(2):
            f = li * 2 + j
            out[f"w_gu{f}"] = _gu_chunks(inp["w_ffn_gate"][li, j], inp["w_ffn_up"][li, j])
            out[f"w_dn{f}"] = _p2_chunks(inp["w_ffn_down"][li, j], 11)
        out[f"w_pg{li}"] = _p2_chunks(inp["w_ple_gate"][li], 8)
        out[f"w_pp{li}"] = np.ascontiguousarray(
            inp["w_ple_proj"][li].reshape(2, 128, D).transpose(1, 0, 2)).reshape(128, 2 * D)
        jj = li // 2
        if li % 2 == 0:
            wi = inp["w_in_even"][jj]
            out[f"w_i1_{li}"] = _p1_chunks(wi[:, 0:2048])
            out[f"w_i2_{li}"] = _p2_chunks(wi[:, 2048:6144], 8)
            out[f"w_o_{li}"] = _p2_chunks(inp["w_out_even"][jj], 8)
            out[f"sgu_wT_{li}"] = np.ascontiguousarray(inp["sgu_w"][jj].transpose(2, 0, 1))
            out[f"sgu_bT_{li}"] = np.ascontiguousarray(inp["sgu_b"][jj].T)
            out[f"sgu_g_{li}"] = np.ascontiguousarray(inp["sgu_gain"][jj]).reshape(1, 1024)
        else:
            wi = inp["w_in_odd"][jj]
            out[f"w_i1_{li}"] = _p1_chunks(wi[:, 1024:3072])
            out[f"w_i2_{li}"] = _p2_chunks(np.concatenate([wi[:, 0:1024], wi[:, 3072:4096]], axis=1), 8)
            out[f"w_o_{li}"] = _p2_chunks(inp["w_out_odd"][jj], 8)
            out[f"pool_w_{li}"] = np.ascontiguousarray(
                inp["pool_w"][jj].reshape(4, 2, 128, 256).transpose(2, 0, 1, 3)).reshape(128, 8, 256)
            out[f"pool_s_{li}"] = np.ascontiguousarray(inp["pool_scale"][jj]).reshape(1, 1024)
    out.update(_constants())
    return out


def _constants():
    c = {}
    j = np.arange(128, dtype=np.float64)
    t = np.arange(S, dtype=np.float64)
    inv = 1.0 / np.power(10000.0, j / 128.0)
    ang = (t[None, :].astype(np.float32) * inv[:, None].astype(np.float32)).astype(np.float32)
    c["c_rot"] = np.stack([np.cos(ang), np.sin(ang)]).astype(np.float32)
    g2 = np.zeros((4, 128, 1024), np.float64)
    sl = np.arange(128)[:, None]
    w = np.arange(1024)[None, :]
    e = w - 384 - sl
    for h in range(4):
        gam = 1.0 - 2.0 ** (-5.0 - h)
        g2[h] = np.where(e >= 0, np.power(gam, np.maximum(e, 0)), 0.0) / 16.0
    c["c_g2"] = g2.astype(np.float32)
    c["c_tri"] = (np.arange(128)[None, :] >= np.arange(128)[:, None]).astype(np.float32)
    dd = np.arange(128)
    inv2 = (1.0 / np.power(np.float32(500000.0), (np.arange(16, dtype=np.float32) / 16.0))).astype(np.float32)
    ang2 = t[None, :].astype(np.float32) * inv2[dd % 16][:, None]
    C = np.where(dd[:, None] < 32, np.cos(ang2), 1.0)
    Sn = np.where(dd[:, None] < 32, np.sin(ang2), 0.0)
    c["c_rot2"] = np.stack([C, Sn]).astype(np.float32)
    perm = np.zeros((128, 128), np.float32)
    for d_ in range(16):
        perm[d_ + 16, d_] = -1.0
        perm[d_, d_ + 16] = 1.0
    c["c_perm"] = perm
    ct = np.zeros((4, 128, TT), np.float32)
    sl_ = np.arange(128)[:, None]
    tl_ = np.arange(TT)[None, :]
    for jj in range(4):
        ct[jj] = np.where((tl_ // 256 == jj // 2) & (128 * jj + sl_ > tl_), NEGB, 0.0)
    c["c_ct"] = ct
    ind = np.zeros((8, 8, 128), np.float32)
    for n in range(8):
        ind[n, n, :] = 1.0
    c["c_ind"] = ind.reshape(8, 8 * 128)
    negm = np.zeros((8, 8), np.float32)
    for qb in range(8):
        negm[qb, qb:] = -1e30
    c["c_negm"] = negm.reshape(1, 64)
    bcon = np.zeros((4, 8), np.float32)
    for qb in range(4):
        bcon[qb, qb + 1:] = NEGB
    c["c_bcon"] = bcon.reshape(1, 32)
    pool = np.zeros((12, 128, 128), np.float64)
    ss_ = np.arange(128)[:, None]
    tt_ = np.arange(128)[None, :]
    for gi, w_ in enumerate((2, 4, 8, 16)):
        band = (ss_ <= tt_) & (ss_ >= tt_ - w_ + 1)
        pool[gi] = np.where(band, 1.0 / w_, 0.0) - (ss_ == tt_)
        cnt = np.minimum(tt_ + 1, w_)
        pool[4 + gi] = np.where(band, 1.0 / cnt, 0.0) - (ss_ == tt_)
        pool[8 + gi] = np.where(ss_ >= tt_ + 129 - w_, 1.0 / w_, 0.0)
    c["c_pool"] = pool.astype(np.float32)
    return c


def run(inputs, n_cores, NB, layers, n_stage=4, trace=False):
    inp = {k: np.asarray(v) for k, v in inputs.items()}
    nc = build_program(NB, layers, n_stage)
    wts = prep_weights(inp, layers)
    in_maps = []
    for c in range(n_cores):
        m = dict(wts)
        m["x"] = np.ascontiguousarray(inp["x"][c * NB:(c + 1) * NB]).reshape(NB * S, D)
        m["p"] = np.ascontiguousarray(inp["p"][:, c * NB:(c + 1) * NB]).reshape(4, NB * S, 256)
        m["norm_gains"] = inp["norm_gains"]
        in_maps.append(m)
    res = run_bass_kernel_spmd(nc, in_maps, core_ids=list(range(n_cores)), trace=trace)
    out = np.stack([r["out"].reshape(NB, S, D) for r in res.results]).reshape(n_cores * NB, S, D)
    return out, res


def kernel(**inputs):
    out, _ = run(inputs, 8, 2, [0, 1, 2, 3])
    return out.astype(np.float32)
```
